# Optimizing a Trainium2 kernel written in Bass

```python
import math
import jax, jax.numpy as jnp
from jax import lax
import numpy as np

D_MODEL = 2048
BATCH = 2
SEQ = 4096
DEPTH = 1

CHUNK = 64
N_META = 16
QB = 128
N_BUCKETS = 32
MAX_DISTANCE = 128

A_HEADS = 8
A_HEAD_DIM = 128
KV_RANK = 256
IDX_HEADS = 16
IDX_DIM = 64
TOPK_MAX = 256

B_HEADS = 8
B_QK_DIM = 64
B_V_DIM = 128

A_WIDTH = A_HEADS * A_HEAD_DIM
B_WIDTH = B_HEADS * B_V_DIM
IN_SIZES = (A_WIDTH, KV_RANK, A_WIDTH, IDX_HEADS * IDX_DIM, IDX_DIM, IDX_HEADS,
            2 * B_HEADS * B_QK_DIM, 2 * B_HEADS * B_QK_DIM, B_WIDTH, B_WIDTH,
            D_MODEL, D_MODEL)
IN_WIDTH = (3 * A_WIDTH + KV_RANK + IDX_HEADS * IDX_DIM + IDX_DIM + IDX_HEADS
            - A_WIDTH + 4 * B_HEADS * B_QK_DIM + 2 * B_WIDTH + 2 * D_MODEL)
EPS = 1e-6

kernel_name = "chunk_causal_dsa_diffattn_gated_hybrid"


def rms_norm(x, g):
    xf = x.astype(jnp.float32)
    y = xf * lax.rsqrt(jnp.mean(xf * xf, axis=-1, keepdims=True) + EPS)
    return (y * g.astype(jnp.float32)).astype(x.dtype)


def layer_norm(x, g, b):
    xf = x.astype(jnp.float32)
    mu = jnp.mean(xf, axis=-1, keepdims=True)
    var = jnp.mean(jnp.square(xf - mu), axis=-1, keepdims=True)
    y = (xf - mu) * lax.rsqrt(var + EPS)
    return (y * g.astype(jnp.float32) + b.astype(jnp.float32)).astype(x.dtype)


def chunk_id(pos):
    return jnp.where(pos < N_META, 0, 1 + (pos - N_META) // CHUNK)


def t5_bucket(rel):
    nb = N_BUCKETS // 2
    max_exact = nb // 2
    ret = jnp.where(rel > 0, nb, 0)
    n = jnp.abs(rel)
    nf = jnp.maximum(n, 1).astype(jnp.float32)
    large = max_exact + (jnp.log(nf / max_exact) / math.log(MAX_DISTANCE / max_exact)
                         * (nb - max_exact)).astype(jnp.int32)
    large = jnp.minimum(large, nb - 1)
    return ret + jnp.where(n < max_exact, n, large)


def hybrid_layer(h, layer, rel_bias, pre_w, w_in, kv_norm_w, w_uk, w_uv, ikn_w, ikn_b,
                 lam_p, subln_w, w_o_a, w_o_b, w_out, post_w):
    Bsz, T, _ = h.shape
    u = rms_norm(h, pre_w)
    proj = u @ w_in
    split_at = np.cumsum(np.array(IN_SIZES))[:-1].tolist()
    q_a, ckv, z_a, iq, ik, iw, q_b, k_b, v_b, z_b, g_a, g_b = jnp.split(proj, split_at, axis=-1)

    q_a = q_a.reshape(Bsz, T, A_HEADS, A_HEAD_DIM)
    ckv = rms_norm(ckv, kv_norm_w)
    iq = iq.reshape(Bsz, T, IDX_HEADS, IDX_DIM)
    ik = layer_norm(ik, ikn_w, ikn_b)
    iw = iw * (IDX_HEADS ** -0.5 * IDX_DIM ** -0.5)

    q_b = q_b.reshape(Bsz, T, B_HEADS, 2, B_QK_DIM)
    k_b = k_b.reshape(Bsz, T, B_HEADS, 2, B_QK_DIM)
    v_b = v_b.reshape(Bsz, T, B_HEADS, B_V_DIM)
    k1, k2 = k_b[..., 0, :], k_b[..., 1, :]
    lam_init = 0.8 - 0.6 * math.exp(-0.3 * layer)
    lp = lam_p.astype(jnp.float32)
    lam = jnp.exp(jnp.sum(lp[0] * lp[1])) - jnp.exp(jnp.sum(lp[2] * lp[3])) + lam_init

    bias_a = rel_bias[:, :A_HEADS]
    bias_b = rel_bias[:, A_HEADS:]

    kpos = jnp.arange(T)
    k_cid = chunk_id(kpos)
    n_blk = -(-T // QB)
    Tp = n_blk * QB
    q_pos = jnp.arange(Tp).reshape(n_blk, QB)
    topk = min(TOPK_MAX, T // 4)

    def to_blocks(a):
        a = jnp.pad(a, [(0, 0), (0, Tp - T)] + [(0, 0)] * (a.ndim - 2))
        return a.reshape((Bsz, n_blk, QB) + a.shape[2:]).swapaxes(0, 1)

    def from_blocks(a):
        return a.swapaxes(0, 1).reshape((Bsz, Tp) + a.shape[3:])[:, :T]

    a_scale = A_HEAD_DIM ** -0.5

    def dsa_block(args):
        q, bq, bw, qp = args
        qc = chunk_id(qp)
        s = jnp.einsum('bqhd,bsd->bqhs', bq, ik)
        score = jnp.einsum('bqhs,bqh->bqs', jax.nn.relu(s), bw).astype(jnp.float32)
        allowed = k_cid[None, :] <= qc[:, None]
        score = jnp.where(allowed[None], score, -jnp.inf)
        _, idx = lax.top_k(score, topk)
        valid = k_cid[idx] <= qc[None, :, None]
        sel = jax.vmap(lambda c, i: c[i])(ckv, idx)
        q_lat = jnp.einsum('bqhd,rhd->bqhr', q, w_uk)
        logits = jnp.einsum('bqhr,bqkr->bqhk', q_lat, sel).astype(jnp.float32) * a_scale
        bias = bias_a[t5_bucket(idx - qp[None, :, None])]
        logits = logits + jnp.swapaxes(bias, 2, 3).astype(jnp.float32)
        logits = jnp.where(valid[:, :, None, :], logits, -jnp.inf)
        p = jax.nn.softmax(logits, axis=-1).astype(sel.dtype)
        o_lat = jnp.einsum('bqhk,bqkr->bqhr', p, sel)
        o = jnp.einsum('bqhr,rhd->bqhd', o_lat, w_uv)
        return o.reshape(Bsz, QB, A_WIDTH)

    b_scale = B_QK_DIM ** -0.5

    def diff_block(args):
        q1, q2, qp = args
        qc = chunk_id(qp)
        allowed = (k_cid[None, :] <= qc[:, None])[None, None]
        bias = bias_b[t5_bucket(kpos[None, :] - qp[:, None])]
        bias = jnp.transpose(bias, (2, 0, 1))[None].astype(jnp.float32)

        def probs(q, k):
            l = jnp.einsum('bqhd,bshd->bhqs', q, k).astype(jnp.float32) * b_scale + bias
            return jax.nn.softmax(jnp.where(allowed, l, -jnp.inf), axis=-1)

        attn = probs(q1, k1) - lam * probs(q2, k2)
        return jnp.einsum('bhqs,bshd->bqhd', attn.astype(v_b.dtype), v_b)

    o_a = from_blocks(lax.map(dsa_block, (to_blocks(q_a), to_blocks(iq), to_blocks(iw), q_pos)))
    o_b = from_blocks(lax.map(diff_block, (to_blocks(q_b[..., 0, :]), to_blocks(q_b[..., 1, :]), q_pos)))
    o_b = (rms_norm(o_b, subln_w) * (1.0 - lam_init)).reshape(Bsz, T, B_WIDTH)

    y_a = (o_a * jax.nn.silu(z_a)) @ w_o_a
    y_b = (o_b * jax.nn.silu(z_b)) @ w_o_b
    mix = jax.nn.sigmoid(g_a) * y_a + jax.nn.sigmoid(g_b) * y_b
    out = mix @ w_out
    return h + rms_norm(out, post_w)


def setup_inputs(seed: int = 0) -> dict:
    key = jax.random.key(seed)
    ks = jax.random.split(key, 18)
    f32 = jnp.float32
    nrm = lambda k, shape, s: jax.random.normal(k, shape, f32) * s
    L = DEPTH
    return {
        "x": nrm(ks[0], (BATCH, SEQ, D_MODEL), 1.0),
        "meta_tokens": nrm(ks[1], (N_META, D_MODEL), 1.0),
        "rel_bias": nrm(ks[2], (N_BUCKETS, A_HEADS + B_HEADS), 0.5),
        "pre_norm_w": 1.0 + nrm(ks[3], (L, D_MODEL), 0.02),
        "w_in": nrm(ks[4], (L, D_MODEL, IN_WIDTH), D_MODEL ** -0.5),
        "kv_norm_w": 1.0 + nrm(ks[5], (L, KV_RANK), 0.02),
        "w_uk": nrm(ks[6], (L, KV_RANK, A_HEADS, A_HEAD_DIM), KV_RANK ** -0.5),
        "w_uv": nrm(ks[7], (L, KV_RANK, A_HEADS, A_HEAD_DIM), KV_RANK ** -0.5),
        "idx_k_norm_w": 1.0 + nrm(ks[8], (L, IDX_DIM), 0.02),
        "idx_k_norm_b": nrm(ks[9], (L, IDX_DIM), 0.02),
        "diff_lambda": nrm(ks[10], (L, 4, B_QK_DIM), 0.1),
        "diff_subln_w": 1.0 + nrm(ks[11], (L, B_V_DIM), 0.02),
        "w_o_a": nrm(ks[12], (L, A_WIDTH, D_MODEL), A_WIDTH ** -0.5),
        "w_o_b": nrm(ks[13], (L, B_WIDTH, D_MODEL), B_WIDTH ** -0.5),
        "w_out": nrm(ks[14], (L, D_MODEL, D_MODEL), D_MODEL ** -0.5),
        "post_norm_w": 1.0 + nrm(ks[15], (L, D_MODEL), 0.02),
    }


def reference(x, meta_tokens, rel_bias, pre_norm_w, w_in, kv_norm_w, w_uk, w_uv,
              idx_k_norm_w, idx_k_norm_b, diff_lambda, diff_subln_w, w_o_a, w_o_b,
              w_out, post_norm_w):
    Bsz = x.shape[0]
    meta = jnp.broadcast_to(meta_tokens[None].astype(x.dtype), (Bsz, N_META, x.shape[-1]))
    h = jnp.concatenate([meta, x], axis=1)
    for l in range(DEPTH):
        h = hybrid_layer(h, l, rel_bias, pre_norm_w[l], w_in[l], kv_norm_w[l], w_uk[l], w_uv[l],
                         idx_k_norm_w[l], idx_k_norm_b[l], diff_lambda[l], diff_subln_w[l],
                         w_o_a[l], w_o_b[l], w_out[l], post_norm_w[l])
    return h[:, N_META:]
```

```python
import numpy as np
from contextlib import ExitStack
import concourse.bass as bass
import concourse.mybir as mybir
from concourse.bass_utils import run_bass_kernel_spmd

F32 = mybir.dt.float32
BF16 = mybir.dt.bfloat16
AF = mybir.ActivationFunctionType
ALU = mybir.AluOpType
AX = mybir.AxisListType

EPS = 1e-6
NEG = -30000.0
NIT = 13
A_SCALE = 128 ** -0.5
B_SCALE = 64 ** -0.5
LAM_INIT = 0.8 - 0.6 * 1.0
IW_SCALE = (16 ** -0.5) * (64 ** -0.5)
SBW = 48640


class Buf:
    __slots__ = ("name", "t", "w", "r")

    def __init__(self, name, t):
        self.name = name
        self.t = t
        self.w = {}
        self.r = {}

    def __getitem__(self, k):
        return self.t[k]


class Op:
    __slots__ = ("eng", "key", "fn", "deps", "sig", "sigval", "dma", "idx")


class Prog:
    ENG = ("pe", "act", "dve", "pool", "sp")

    def __init__(self, nc):
        self.nc = nc
        self.ops = {e: [] for e in self.ENG}
        self.latest = {}
        self.bar = {}
        self.chan_cnt = {}
        self.n = 0

    def barrier(self):
        self.bar = dict(self.latest)

    def add(self, eng, fn, reads=(), writes=(), chan=None):
        op = Op()
        op.eng = eng
        op.dma = chan is not None
        op.key = chan if chan is not None else eng
        op.fn = fn
        op.sig = op.dma
        op.sigval = None
        op.idx = self.n
        self.n += 1
        deps = {}

        def dep(o):
            if o.key == op.key and eng == "pe" and not op.dma:
                return
            p = deps.get(o.key)
            if p is None or p.idx < o.idx:
                deps[o.key] = o

        for o in self.bar.values():
            dep(o)
        for b in reads:
            for o in b.w.values():
                dep(o)
        for b in writes:
            for o in b.w.values():
                dep(o)
            for o in b.r.values():
                dep(o)
        op.deps = list(deps.values())
        for o in op.deps:
            o.sig = True
        for b in reads:
            b.r[op.key] = op
        for b in writes:
            b.w[op.key] = op
            b.r = {}
        if op.dma:
            c = self.chan_cnt.get(chan, 0) + 1
            self.chan_cnt[chan] = c
            op.sigval = 16 * c
        self.latest[op.key] = op
        self.ops[eng].append(op)
        return op

    def emit(self, es):
        nc = self.nc
        for e in self.ENG:
            c = 0
            for op in self.ops[e]:
                if op.dma:
                    continue
                if op.sig:
                    c += 1
                    op.sigval = c
        keys = set()
        for e in self.ENG:
            for op in self.ops[e]:
                if op.sig:
                    keys.add(op.key)
        sems = {}
        for k in sorted(keys):
            sems[k] = es.enter_context(nc.semaphore("s_" + k))
        block = es.enter_context(nc.Block())

        def run(e, h):
            waited = {}
            for op in self.ops[e]:
                for d in op.deps:
                    if waited.get(d.key, 0) < d.sigval:
                        h.wait_ge(sems[d.key], d.sigval)
                        waited[d.key] = d.sigval
                ins = op.fn(h)
                if op.sig:
                    ins.then_inc(sems[op.key], 16 if op.dma else 1)
            if e in ("sp", "pool"):
                for k, o in self.latest.items():
                    if o.sig and waited.get(k, 0) < o.sigval:
                        h.wait_ge(sems[k], o.sigval)
                        waited[k] = o.sigval

        @block.tensor
        def _(h):
            run("pe", h)

        @block.scalar
        def _(h):
            run("act", h)

        @block.vector
        def _(h):
            run("dve", h)

        @block.gpsimd
        def _(h):
            run("pool", h)

        @block.sync
        def _(h):
            run("sp", h)


class Arena:
    def __init__(self, ap, nwords):
        self.ap = ap
        self.n = nwords
        self.used = []

    def alloc(self, name, free_shape, dt, parts=128):
        ne = int(np.prod(free_shape))
        nw = (ne * (2 if dt == BF16 else 4) + 3) // 4
        nw = (nw + 7) // 8 * 8
        self.used.sort()
        pos = 0
        for (s, z, _) in self.used:
            if s - pos >= nw:
                break
            pos = max(pos, s + z)
        assert pos + nw <= self.n, f"SBUF arena OOM for {name}: need {nw} at {pos}, used={sum(z for _, z, _ in self.used)}"
        self.used.append((pos, nw, name))
        v = self.ap[0:parts, pos:pos + nw]
        if dt == BF16:
            v = v.bitcast(BF16)
        v = v[:, 0:ne]
        if len(free_shape) == 2:
            v = v.rearrange("p (a b) -> p a b", a=free_shape[0])
        elif len(free_shape) == 3:
            v = v.rearrange("p (a b c) -> p a b c", a=free_shape[0], b=free_shape[1])
        return Buf(name, v)

    def free(self, *bufs):
        names = {b.name for b in bufs}
        self.used = [u for u in self.used if u[2] not in names]


def build_program(dbg=()):
    nc = bass.Bass("TRN2", target_bir_lowering=False)

    def din(name, shape, dt=F32):
        return nc.dram_tensor(name, list(shape), dt, kind="ExternalInput").ap()

    xk = din("xk", [4224, 2048])
    xq = din("xq", [1024, 2048])
    wk = din("wk", [2048, 2368])
    wq = din("wq", [72 * 128, 2048])
    wiw = din("wiw", [2048, 16])
    wukT = din("wukT", [128, 2048])
    wuv = din("wuv", [256, 1024])
    woa = din("woa", [16 * 128, 1024])
    wob = din("wob", [16 * 128, 1024])
    wout = din("wout", [2048, 2048])
    prewT = din("prewT", [128, 16])
    kvw = din("kvw", [128, 256])
    ikw = din("ikw", [128, 128])
    ikb = din("ikb", [128, 128])
    lamp = din("lamp", [128, 256])
    subw = din("subw", [128, 1])
    postw = din("postw", [128, 2048])
    ident = din("ident", [128, 128])
    bta = din("bta", [128, 5 * 8 * 128])
    btb = din("btb", [128, 8 * 5 * 128])
    ca = din("ca", [128, 8])
    cb = din("cb", [128, 8])
    btam = din("btam", [16, 8 * 128])
    btbm = din("btbm", [16, 8 * 128])
    madd = din("madd", [128, 5 * 128])
    mk = din("mk", [128, 512])
    y = nc.dram_tensor("y", [1024, 2048], F32, kind="ExternalOutput").ap()
    kT_d = nc.dram_tensor("kT_d", [8, 128, 4224], BF16, kind="Internal").ap()
    V_d = nc.dram_tensor("V_d", [4224, 1024], BF16, kind="Internal").ap()
    uTo_d = nc.dram_tensor("uTo_d", [128, 16 * 1024], BF16, kind="Internal").ap()
    kT_db = Buf("kT_d", kT_d)
    V_db = Buf("V_d", V_d)
    uTo_db = Buf("uTo_d", uTo_d)
    qlat_d = nc.dram_tensor("qlat_d", [128, 2 * 8 * 1024], BF16, kind="Internal").ap()
    qlat_db = Buf("qlat_d", qlat_d)
    dbg_out = {}
    for name, shape in dbg:
        dbg_out[name] = nc.dram_tensor(name, list(shape), F32, kind="ExternalOutput").ap()

    P = Prog(nc)
    with ExitStack() as es:
        arena_ap = es.enter_context(nc.sbuf_tensor("arena", [128, SBW], F32))
        ps_ap = es.enter_context(nc.psum_tensor("psarena", [128, 4096], F32))
        AR = Arena(arena_ap, SBW)
        sb = AR.alloc
        pb = [Buf("pb%d" % i, ps_ap[:, 512 * i:512 * (i + 1)]) for i in range(8)]

        def psv(b0, nb=1):
            return ps_ap[:, 512 * b0:512 * (b0 + nb)]

        def psbf(b0, nb=1):
            return ps_ap[:, 512 * b0:512 * (b0 + nb)].bitcast(BF16)

        def pbs(b0, nb=1):
            return [pb[b0 + i] for i in range(nb)]

        def dma(out_ap, in_ap, reads, writes, chan):
            P.add("sp", lambda e: e.dma_start(out=out_ap, in_=in_ap), reads=reads, writes=writes, chan=chan)

        def dma2(q, out_ap, in_ap, reads, writes, chan):
            P.add("sp" if q % 2 == 0 else "pool", lambda e: e.dma_start(out=out_ap, in_=in_ap), reads=reads, writes=writes, chan=chan)

        def dmas(out_ap, in_ap, reads, writes, chan):
            P.add("pool", lambda e: e.dma_start(out=out_ap, in_=in_ap), reads=reads, writes=writes, chan=chan)

        def mm(out_ap, lhsT, rhs, start, stop, reads, writes):
            P.add("pe", lambda e: e.matmul(out_ap, lhsT=lhsT, rhs=rhs, start=start, stop=stop), reads=reads, writes=writes)

        def tr(out_ap, in_ap, reads, writes):
            P.add("pe", lambda e: e.transpose(out=out_ap, in_=in_ap, identity=identb[:]), reads=list(reads) + [identb], writes=writes)

        def act(out_ap, in_ap, func, reads, writes, **kw):
            P.add("act", lambda e: e.activation(out=out_ap, in_=in_ap, func=func, **kw), reads=reads, writes=writes)

        def ts(eng, out_ap, in0, s1, s2, op0, op1, reads, writes, accum_out=None):
            if accum_out is not None:
                P.add(eng, lambda e: e.tensor_scalar(out=out_ap, in0=in0, scalar1=s1, scalar2=s2, op0=op0, op1=op1, accum_out=accum_out), reads=reads, writes=writes)
            elif op1 is None:
                P.add(eng, lambda e: e.tensor_scalar(out=out_ap, in0=in0, scalar1=s1, scalar2=None, op0=op0), reads=reads, writes=writes)
            else:
                P.add(eng, lambda e: e.tensor_scalar(out=out_ap, in0=in0, scalar1=s1, scalar2=s2, op0=op0, op1=op1), reads=reads, writes=writes)

        def tt(eng, out_ap, in0, in1, op, reads, writes):
            P.add(eng, lambda e: e.tensor_tensor(out=out_ap, in0=in0, in1=in1, op=op), reads=reads, writes=writes)

        def stt(out_ap, in0, scalar, in1, op0, op1, reads, writes):
            P.add("dve", lambda e: e.scalar_tensor_tensor(out=out_ap, in0=in0, scalar=scalar, in1=in1, op0=op0, op1=op1), reads=reads, writes=writes)

        def cp(eng, out_ap, in_ap, reads, writes):
            if eng == "act":
                act(out_ap, in_ap, AF.Copy, reads, writes)
            else:
                P.add(eng, lambda e: e.tensor_copy(out=out_ap, in_=in_ap), reads=reads, writes=writes)

        def recip(out_ap, in_ap, reads, writes):
            P.add("dve", lambda e: e.reciprocal(out=out_ap, in_=in_ap), reads=reads, writes=writes)

        def memset(eng, ap, val, writes):
            P.add(eng, lambda e: e.memset(ap, val), writes=writes)

        def dbg_dump(name, src_ap, reads):
            if name in dbg_out:
                dma(dbg_out[name], src_ap, reads, [], "dbg_" + name)

        identf = sb("identf", [128], F32)
        identb = sb("identb", [128], BF16)
        onesb = sb("onesb", [128], BF16)
        prew = sb("prew", [16], F32)
        epsb = sb("epsb", [1], F32)
        junk = sb("junk", [2048], BF16)
        dma(identf[:], ident, [], [identf], "identf")
        cp("dve", identb[:], identf[:], [identf], [identb])
        memset("pool", onesb[:], 1.0, [onesb])
        memset("pool", epsb[:], EPS, [epsb])
        dma(prew[:], prewT, [], [prew], "prew")

        ikT2 = sb("ikT2", [4112], BF16)
        ckvT = sb("ckvT", [2, 4112], BF16)
        ckvtm = sb("ckvtm", [33, 256], BF16)

        def rmsnorm_a(xt, st, ub):
            P.add("act", lambda e: e.activation(out=junk[:], in_=xt[:], func=AF.Square, accum_out=st[:, 0:1]), reads=[xt], writes=[st])
            act(st[:, 1:2], st[:, 0:1], AF.Sqrt, [st, epsb], [st], scale=1.0 / 2048, bias=epsb[:])
            recip(st[:, 2:3], st[:, 1:2], [st], [st])
            ts("dve", ub[:], xt[:], st[:, 2:3], None, ALU.mult, None, [xt, st], [ub])

        def rmsnorm_b(ub, dst_ap, dst_bufs):
            for c in range(16):
                tr(psbf(0, 2)[:, c * 128:(c + 1) * 128], ub[:, c * 128:(c + 1) * 128], [ub], pbs(0, 2))
            tt("dve", dst_ap, psbf(0, 2).rearrange("p (c t) -> p c t", c=16), prew[:].unsqueeze(2).to_broadcast([128, 16, 128]),
               ALU.mult, pbs(0, 2) + [prew], dst_bufs)

        def rmsnorm_T(xt, st, ub, dst_ap, dst_bufs):
            rmsnorm_a(xt, st, ub)
            rmsnorm_b(ub, dst_ap, dst_bufs)

        wkb = sb("wkb", [16, 2368], BF16)
        wst = [sb("wst%d" % i, [1184], F32) for i in range(2)]
        kvwt = sb("kvwt", [256], F32)
        ikwt = sb("ikwt", [2, 64], F32)
        ikbt = sb("ikbt", [2, 64], F32)
        dma(kvwt[:], kvw, [], [kvwt], "kvwt")
        dma(ikwt[:], ikw.rearrange("p (a b) -> p a b", a=2), [], [ikwt], "ikwt")
        dma(ikbt[:], ikb.rearrange("p (a b) -> p a b", a=2), [], [ikbt], "ikbt")
        for c2 in range(32):
            c, hf = c2 // 2, c2 % 2
            s = wst[c2 % 2]
            dma2(c2, s[:], wk[c * 128:(c + 1) * 128, hf * 1184:(hf + 1) * 1184], [], [s], s.name)
            cp("act" if c2 % 2 == 0 else "dve", wkb[:, c, hf * 1184:(hf + 1) * 1184], s[:], [s], [wkb])
        xts = [sb("xt%d" % i, [2048], F32) for i in range(2)]
        ubs = [sb("ub%d" % i, [2048], BF16) for i in range(2)]
        uTs = [sb("uT%d" % i, [16, 128], BF16) for i in range(2)]
        sts = [sb("st%d" % i, [16], F32) for i in range(2)]
        st2s = [sb("st2%d" % i, [16], F32) for i in range(2)]
        ckik = [sb("ckik%d" % i, [320], F32) for i in range(2)]
        kbtm = [sb("kbtm%d" % i, [1024], BF16) for i in range(2)]
        kTblk = [sb("kTblk%d" % i, [8, 128], BF16) for i in range(2)]
        vtm = [sb("vtm%d" % i, [1024], BF16) for i in range(2)]
        ikc = sb("ikc", [64], F32)
        ikdf = sb("ikdf", [2, 64], F32)
        ikds = [sb("ikd%d" % i, [2, 64], BF16) for i in range(2)]
        GRP = [(0, 320), (320, 512), (832, 512), (1344, 512), (1856, 512)]

        def front0a(tb):
            xt, ub, st = xts[tb % 2], ubs[tb % 2], sts[tb % 2]
            dma(xt[:], xk[tb * 128:(tb + 1) * 128, :], [], [xt], xt.name)
            rmsnorm_a(xt, st, ub)

        def front0b(tb):
            rmsnorm_b(ubs[tb % 2], uTs[tb % 2][:], [uTs[tb % 2]])

        def post0(tb):
            nv = 128 if tb < 32 else 16
            col0 = tb * 128
            ikd, kb_, kt = ikds[tb % 2], kbtm[tb % 2], kTblk[tb % 2]
            pk = psbf(7)
            for rc in range(2):
                tr(pk[:, 512 + rc * 128:512 + (rc + 1) * 128], ckvtm[:, tb, rc * 128:(rc + 1) * 128], [ckvtm], [pb[7]])
            tr(pk[:, 768:896], ikd[:].rearrange("p a b -> p (a b)"), [ikd], [pb[7]])
            cp("act", ckvT[:, :, col0:col0 + nv], pk[:, 512:768].rearrange("p (a b) -> p a b", a=2)[:, :, 0:nv], [pb[7]], [ckvT])
            cp("act", ikT2[:, col0:col0 + nv], pk[:, 768:768 + nv], [pb[7]], [ikT2])
            for half in range(2):
                for q in range(4):
                    hh = half * 4 + q
                    tr(pk[:, q * 128:(q + 1) * 128], kb_[:, hh * 128:(hh + 1) * 128], [kb_], [pb[7]])
                cp("dve" if half == 0 else "act", kt[:, half * 4:(half + 1) * 4, :], pk[:, 0:512].rearrange("p (a b) -> p a b", a=4), [pb[7]], [kt])
            dmas(kT_d.rearrange("h p n -> p h n")[:, :, col0:col0 + 128], kt[:], [kt], [kT_db], kt.name)

        front0a(0)
        front0b(0)
        for tb in range(33):
            uT, st, ck, kb_, vt, ikd = uTs[tb % 2], st2s[tb % 2], ckik[tb % 2], kbtm[tb % 2], vtm[tb % 2], ikds[tb % 2]
            col0 = tb * 128
            if tb + 1 < 33:
                front0a(tb + 1)
            for c in range(16):
                if c == 8 and tb + 1 < 33:
                    front0b(tb + 1)
                for n, (n0, w) in enumerate(GRP):
                    mm(pb[2 + n][:, 0:w], uT[:, c, :], wkb[:, c, n0:n0 + w], c == 0, c == 15, [uT, wkb], [pb[2 + n]])
            cp("act", ck[:], pb[2][:, 0:320], [pb[2]], [ck])
            cp("act", kb_[:], psv(3, 2), pbs(3, 2), [kb_])
            cp("dve", vt[:], psv(5, 2), pbs(5, 2), [vt])
            dmas(V_d[col0:col0 + 128, :], vt[:], [vt], [V_db], vt.name)
            if tb > 0:
                post0(tb - 1)
            P.add("act", lambda e, st=st, ck=ck: e.activation(out=junk[:, 0:256], in_=ck[:, 0:256], func=AF.Square, accum_out=st[:, 3:4]), reads=[ck], writes=[st])
            act(st[:, 4:5], st[:, 3:4], AF.Sqrt, [st, epsb], [st], scale=1.0 / 256, bias=epsb[:])
            P.add("act", lambda e, st=st, ck=ck: e.activation(out=junk[:, 256:320], in_=ck[:, 256:320], func=AF.Copy, accum_out=st[:, 6:7]), reads=[ck], writes=[st])
            P.add("act", lambda e, st=st, ck=ck: e.activation(out=junk[:, 320:384], in_=ck[:, 256:320], func=AF.Square, accum_out=st[:, 7:8]), reads=[ck], writes=[st])
            recip(st[:, 5:6], st[:, 4:5], [st], [st])
            stt(ckvtm[:, tb, :], ck[:, 0:256], st[:, 5:6], kvwt[:], ALU.mult, ALU.mult, [ck, st, kvwt], [ckvtm])
            ts("dve", st[:, 8:9], st[:, 6:7], 1.0 / 64, None, ALU.mult, None, [st], [st])
            tt("dve", st[:, 9:10], st[:, 8:9], st[:, 8:9], ALU.mult, [st], [st])
            stt(st[:, 10:11], st[:, 7:8], 1.0 / 64, st[:, 9:10], ALU.mult, ALU.subtract, [st], [st])
            act(st[:, 11:12], st[:, 10:11], AF.Sqrt, [st, epsb], [st], scale=1.0, bias=epsb[:])
            recip(st[:, 12:13], st[:, 11:12], [st], [st])
            ts("dve", ikc[:], ck[:, 256:320], st[:, 8:9], st[:, 12:13], ALU.subtract, ALU.mult, [ck, st], [ikc])
            tt("dve", ikdf[:], ikc[:].unsqueeze(1).to_broadcast([128, 2, 64]), ikwt[:], ALU.mult, [ikc, ikwt], [ikdf])
            tt("dve", ikd[:], ikdf[:], ikbt[:], ALU.add, [ikdf, ikbt], [ikd])
        post0(32)
        P.barrier()
        AR.free(wkb, *wst, kvwt, ikwt, ikbt, *xts, *ubs, *uTs, *sts, *st2s, *ckik, *kbtm, *kTblk, *vtm, ikc, ikdf, *ikds)
        if "d_ckv" in dbg_out:
            tmpd = sb("tmpd", [33, 256], F32)
            cp("dve", tmpd[:], ckvtm[:], [ckvtm], [tmpd])
            dma(dbg_out["d_ckv"], tmpd[:], [tmpd], [], "dbg1")
            tmpe = sb("tmpe", [4112], F32)
            cp("dve", tmpe[:], ikT2[:], [ikT2], [tmpe])
            dma(dbg_out["d_ikT"], tmpe[:], [tmpe], [], "dbg2")
            tmpf_ = sb("tmpf_", [2, 4112], F32)
            cp("dve", tmpf_[:], ckvT[:], [ckvT], [tmpf_])
            dma(dbg_out["d_ckvT"], tmpf_[:], [tmpf_], [], "dbg3")
        if "stop0" in dbg_out:
            P.emit(es)
            return nc

        def alloc_proj():
            d = {}
            d["wstc"] = [sb("wstc%d" % i, [16, 128], F32) for i in range(3)]
            d["wbfc"] = [sb("wbfc%d" % i, [16, 128], BF16) for i in range(2)]
            d["k"] = 0
            return d

        def free_proj(d):
            AR.free(*d["wstc"], *d["wbfc"])

        def proj_prefetch(d, chunk):
            k = d["k"]
            d["k"] += 1
            s, w = d["wstc"][k % 3], d["wbfc"][k % 2]
            dma2(k, s[:], wq[chunk * 128:(chunk + 1) * 128, :].rearrange("p (c f) -> p c f", c=16), [], [s], s.name)
            cp("act" if k % 2 == 0 else "dve", w[:], s[:], [s], [w])
            return (k, w)

        def proj_chunk(d, uT, chunk, evac, bank_sets=((0, 1), (2, 3)), pre=None):
            k, w = pre if pre is not None else proj_prefetch(d, chunk)
            bs = bank_sets[k % len(bank_sets)]
            for half in range(2):
                for c in range(16):
                    mm(pb[bs[half]][:, :], w[:, c, :], uT[:, c, half * 512:(half + 1) * 512], c == 0, c == 15, [w, uT], [pb[bs[half]]])
            evac(bs)

        iqm = [sb("iqm%d" % i, [8, 1024], BF16) for i in range(2)]
        memset("dve", iqm[0][64:128], 0.0, [iqm[0]])
        memset("dve", iqm[1][0:64], 0.0, [iqm[1]])
        iwS = sb("iwS", [8, 16], F32)
        uTo = sb("uTo", [16, 1024], BF16)
        qlatT = sb("qlatT", [2, 8, 1024], BF16)
        xts = [sb("xt%d" % i, [2048], F32) for i in range(2)]
        ubs = [sb("ub%d" % i, [2048], BF16) for i in range(2)]
        sts = [sb("st%d" % i, [16], F32) for i in range(2)]
        wiws = sb("wiws", [16, 16], F32)
        wiwb = sb("wiwb", [16, 16], BF16)
        P.add("sp", lambda e: e.dma_start(out=wiws[:], in_=wiw.rearrange("(c p) f -> p c f", p=128)), reads=[], writes=[wiws], chan="wiws")
        cp("dve", wiwb[:], wiws[:], [wiws], [wiwb])
        for j in range(8):
            xt, ub, st = xts[j % 2], ubs[j % 2], sts[j % 2]
            dma(xt[:], xq[j * 128:(j + 1) * 128, :], [], [xt], xt.name)
            rmsnorm_T(xt, st, ub, uTo[:, :, j * 128:(j + 1) * 128], [uTo])
        dmas(uTo_d, uTo[:].rearrange("p c t -> p (c t)"), [uTo], [uTo_db], "uTo_st")
        for j in range(8):
            for c in range(16):
                mm(pb[7][:, j * 16:(j + 1) * 16], uTo[:, c, j * 128:(j + 1) * 128], wiwb[:, c, :], c == 0, c == 15, [uTo, wiwb], [pb[7]])
        act(iwS[:], pb[7][:, 0:128].rearrange("p (a b) -> p a b", a=8), AF.Copy, [pb[7]], [iwS], scale=IW_SCALE)
        P.barrier()
        AR.free(*xts, *ubs, *sts, wiws, wiwb)
        pj = alloc_proj()
        for hp in range(8):
            def ev(bs, hp=hp):
                for half in range(2):
                    cp("act", iqm[0][0:64, hp, half * 512:(half + 1) * 512], pb[bs[half]][0:64, :], [pb[bs[half]]], [iqm[0]])
                    cp("dve", iqm[1][64:128, hp, half * 512:(half + 1) * 512], pb[bs[half]][64:128, :], [pb[bs[half]]], [iqm[1]])
            proj_chunk(pj, uTo, hp, ev)
        wukb = sb("wukb", [8, 256], BF16)
        wuks = pj["wstc"][2]
        dma(wuks[:].rearrange("p a b -> p (a b)"), wukT, [], [wuks], wuks.name)
        cp("dve", wukb[:], wuks[:].rearrange("p a b -> p (a b)").rearrange("p (h r) -> p h r", h=8), [wuks], [wukb])
        qaT = [sb("qaT%d" % i, [1024], BF16) for i in range(2)]
        pend_ql = []
        for h in range(8):
            def ev(bs, h=h):
                q = qaT[h % 2]
                for half in range(2):
                    cp("act" if half == 0 else "dve", q[:, half * 512:(half + 1) * 512], pb[bs[half]][:, :], [pb[bs[half]]], [q])

                def ql():
                    for rc in range(2):
                        for half in range(2):
                            bk = 4 + rc * 2 + half
                            mm(pb[bk][:, :], wukb[:, h, rc * 128:(rc + 1) * 128], q[:, half * 512:(half + 1) * 512], True, True, [wukb, q], [pb[bk]])
                        cp("act" if rc == 0 else "dve", qlatT[:, rc, h, :], psv(4 + rc * 2, 2), pbs(4 + rc * 2, 2), [qlatT])
                pend_ql.append(ql)
            proj_chunk(pj, uTo, 16 + h, ev)
            if len(pend_ql) > 1:
                pend_ql.pop(0)()
        while pend_ql:
            pend_ql.pop(0)()
        dmas(qlat_d, qlatT[:].rearrange("p a b c -> p (a b c)"), [qlatT], [qlat_db], "qlat_st")
        P.barrier()
        free_proj(pj)
        AR.free(uTo, qlatT, wukb, *qaT)

        maskT = sb("maskT", [144, 128], BF16)
        maskTm = sb("maskTm", [8, 128], BF16)
        score = [sb("score%d" % i, [16 + 4096], F32) for i in range(2)]
        rb = [sb("rb%d" % i, [512], BF16) for i in range(4)]
        diag = [sb("diag%d" % i, [16, 128], BF16) for i in range(2)]
        selb = [sb("selb%d" % i, [16 + 4096], BF16) for i in range(2)]
        mkt = sb("mkt", [512], F32)
        tbs = [sb("tb%d" % i, [8], F32) for i in range(2)]
        dma(mkt[:], mk, [], [mkt], "mkt")
        sidx = 0
        gidx = 0

        def scoring_units(j):
            units = []
            dg = diag[j % 2]
            sc = score[j % 2]

            def u0():
                tt("dve", dg[:], identb[:].unsqueeze(1).to_broadcast([128, 16, 128]),
                   iwS[:, j, :].unsqueeze(2).to_broadcast([128, 16, 128]), ALU.mult, [identb, iwS], [dg])
            units.append(u0)
            for g in range(j + 2):
                def ug(g=g):
                    nonlocal sidx, gidx
                    if g <= j:
                        c0, w, d0 = g * 512, 512, 16 + g * 512
                    else:
                        c0, w, d0 = 4096, 16, 0
                    scb = 4 + (gidx % 2)
                    gidx += 1
                    pend = []

                    def qk(h):
                        nonlocal sidx
                        hp, hh = h // 2, h % 2
                        bk = sidx % 4
                        r = rb[sidx % 4]
                        sidx += 1
                        mm(pb[bk][:, 0:w], iqm[hh][:, hp, j * 128:(j + 1) * 128],
                           ikT2[:, c0:c0 + w], True, True, [iqm[hh], ikT2], [pb[bk]])
                        act(r[:, 0:w], pb[bk][:, 0:w], AF.Relu, [pb[bk]], [r])
                        pend.append((h, r))

                    def dgm():
                        h, r = pend.pop(0)
                        mm(pb[scb][:, 0:w], dg[:, h, :], r[:, 0:w], h == 0, h == 15, [dg, r], [pb[scb]])

                    for h in range(16):
                        qk(h)
                        if h >= 2:
                            dgm()
                    dgm()
                    dgm()
                    if g == j:
                        tt("dve", sc[:, d0:d0 + w], pb[scb][:, 0:w], mkt[:], ALU.add, [pb[scb], mkt], [sc])
                    else:
                        cp("act", sc[:, d0:d0 + w], pb[scb][:, 0:w], [pb[scb]], [sc])
                units.append(ug)
            return units

        def bisect_units(j):
            units = []
            sc = score[j % 2]
            n = 16 + (j + 1) * 512
            tbv = tbs[j % 2]
            sl = selb[j % 2]
            units.append(lambda: memset("dve", tbv[:, 0:1], 0.0, [tbv]))
            for k in range(NIT):
                def uk(k=k):
                    step = 3.0 / (2 ** k)
                    P.add("dve", lambda e: e.tensor_scalar(
                        out=sl[:, 0:n], in0=sc[:, 0:n], scalar1=tbv[:, 0:1], scalar2=None, op0=ALU.is_gt, op1=ALU.add,
                        accum_out=tbv[:, 1:2]), reads=[sc, tbv], writes=[tbv, sl])
                    ts("dve", tbv[:, 2:3], tbv[:, 1:2], 255.5, 2.0 * step, ALU.is_ge, ALU.mult, [tbv], [tbv])
                    stt(tbv[:, 0:1], tbv[:, 2:3], -step, tbv[:, 0:1], ALU.add, ALU.add, [tbv], [tbv])
                units.append(uk)

            def ufin():
                ts("dve", tbv[:, 3:4], tbv[:, 0:1], -3.0 / (2 ** (NIT - 1)), None, ALU.add, None, [tbv], [tbv])
                ts("dve", sl[:, 0:n], sc[:, 0:n], tbv[:, 3:4], None, ALU.is_gt, None, [sc, tbv], [sl])
                if j == 1 and "d_score" in dbg_out:
                    dma(dbg_out["d_score"], sc[:, 0:16 + 1024], [sc], [], "dbgs")
                    dma(dbg_out["d_thr"], tbv[:], [tbv], [], "dbgt")
                base = 2 * j * j + 2 * j
                nkb = 4 * j + 4
                tr(psbf(6)[0:16, 0:128], sl[:, 0:16], [sl], [pb[6]])
                cp("act", maskTm[0:16, j, :], psbf(6)[0:16, 0:128], [pb[6]], [maskTm])
                for k0 in range(0, nkb, 8):
                    kn = min(8, nkb - k0)
                    bk = 6 + ((k0 // 8) % 2)
                    for q in range(kn):
                        kb = k0 + q
                        tr(psbf(bk)[:, q * 128:(q + 1) * 128], sl[:, 16 + kb * 128:16 + (kb + 1) * 128], [sl], [pb[bk]])
                    cp("act", maskT[:, base + k0:base + k0 + kn, :],
                       psbf(bk)[:, 0:kn * 128].rearrange("p (a b) -> p a b", a=kn), [pb[bk]], [maskT])
            units.append(ufin)
            return units

        prev = []
        for jj in range(9):
            j = 7 - jj
            su = scoring_units(j) if jj < 8 else []
            bu = prev
            if su:
                per = -(-len(bu) // len(su))
                for u in su:
                    u()
                    for _ in range(per):
                        if bu:
                            bu.pop(0)()
            while bu:
                bu.pop(0)()
            prev = bisect_units(j) if jj < 8 else []
        P.barrier()
        AR.free(*score, *rb, *diag, *selb, mkt, *tbs, *iqm, iwS, ikT2)
        if "stop2" in dbg_out:
            P.emit(es)
            return nc

        oaT = sb("oaT", [8, 1024], BF16)
        qlatT = sb("qlatT", [2, 8, 1024], BF16)
        dma(qlatT[:].rearrange("p a b c -> p (a b c)"), qlat_d, [qlat_db], [qlatT], "qlat_ld")
        btat = sb("btat", [5, 8, 128], F32)
        btamt = sb("btamt", [8, 128], F32)
        cat = sb("cat", [8], F32)
        wuvs = sb("wuvs", [2, 1024], F32)
        wuvb = sb("wuvb", [2, 8, 128], BF16)
        dma(btat[:].rearrange("p a b c -> p (a b c)"), bta, [], [btat], "btat")
        dma(btamt[0:16].rearrange("p a b -> p (a b)"), btam, [], [btamt], "btamt")
        dma(cat[:], ca, [], [cat], "cat")
        dma(wuvs[:], wuv.rearrange("(rc p) n -> p rc n", p=128), [], [wuvs], "wuvs")
        cp("dve", wuvb[:].rearrange("p a b c -> p a (b c)"), wuvs[:], [wuvs], [wuvb])
        for r in range(5):
            tt("dve", btat[:, r, :, :], btat[:, r, :, :], cat[:].unsqueeze(2).to_broadcast([128, 8, 128]), ALU.subtract, [btat, cat], [btat])
        tt("dve", btamt[0:16], btamt[0:16], cat[0:16].unsqueeze(2).to_broadcast([16, 8, 128]), ALU.subtract, [btamt, cat], [btamt])
        act(btat[:], btat[:], AF.Exp, [btat], [btat])
        act(btamt[0:16], btamt[0:16], AF.Exp, [btamt], [btamt])
        DA = 3
        pts = [sb("pt%d" % i, [4, 128], BF16) for i in range(DA + 2)]
        tmpf = [sb("tmpf%d" % i, [4, 128], BF16) for i in range(3)]
        rcp = sb("rcp", [512], F32)
        olat = sb("olat", [2, 1024], BF16)
        uidx = 0
        tfidx = 0
        LB_A = [3, 4, 5, 6, 7]
        lrot = 0
        for j in range(8):
            nkb = 4 * j + 4
            base = 2 * j * j + 2 * j
            for hg in range(2):
                units = list(range(nkb)) + [-1]
                nun = len(units)
                state = {}

                def qk_unit(u):
                    nonlocal uidx, tfidx, lrot
                    kb = units[u]
                    if kb >= 0:
                        KS, c0 = 128, kb * 128
                        rr = kb - (4 * j - 1)
                        near = btat[:, rr, hg * 4:(hg + 1) * 4, :] if rr >= 0 else None
                        mT = maskT[:, base + kb, :]
                        mbuf = maskT
                    else:
                        KS, c0 = 16, 4096
                        near = btamt[0:16, hg * 4:(hg + 1) * 4, :] if j == 0 else None
                        mT = maskTm[0:16, j, :]
                        mbuf = maskTm
                    Lb = LB_A[lrot % 5]
                    lrot += 1
                    for rc in range(2):
                        mm(pb[Lb][0:KS, :].rearrange("p (a b) -> p a b", a=4), ckvT[:, rc, c0:c0 + KS], qlatT[:, rc, hg * 4:(hg + 1) * 4, j * 128:(j + 1) * 128],
                           rc == 0, rc == 1, [ckvT, qlatT], [pb[Lb]])
                    pt = pts[uidx % (DA + 2)]
                    uidx += 1
                    Lv = pb[Lb][0:KS, :].rearrange("p (a b) -> p a b", a=4)
                    act(pt[0:KS], Lv, AF.Exp, [pb[Lb]], [pt], scale=A_SCALE)
                    if near is not None:
                        tf = tmpf[tfidx % 3]
                        tfidx += 1
                        tt("dve", tf[0:KS], near, mT.unsqueeze(1).to_broadcast([KS, 4, 128]), ALU.mult, [btat, btamt, mbuf], [tf])
                        tt("dve", pt[0:KS], pt[0:KS], tf[0:KS], ALU.mult, [pt, tf], [pt])
                    else:
                        tt("dve", pt[0:KS], pt[0:KS], mT.unsqueeze(1).to_broadcast([KS, 4, 128]), ALU.mult, [pt, mbuf], [pt])
                    state[u] = (pt, KS, kb)

                def pv_unit(u):
                    pt, KS, kb = state.pop(u)
                    kbi = kb if kb >= 0 else 32
                    first = u == 0
                    last = u == nun - 1
                    rhs = pt[0:KS].rearrange("p a b -> p (a b)")
                    for rc in range(2):
                        mm(pb[rc][:, :], ckvtm[0:KS, kbi, rc * 128:(rc + 1) * 128], rhs, first, last, [ckvtm, pt], [pb[rc]])
                    mm(pb[2][:, :], onesb[0:KS, :], rhs, first, last, [onesb, pt], [pb[2]])

                for u in range(min(DA, nun)):
                    qk_unit(u)
                for u in range(nun):
                    if u + DA < nun:
                        qk_unit(u + DA)
                    pv_unit(u)
                act(rcp[:], pb[2][:, :], AF.Ln, [pb[2]], [rcp])
                act(rcp[:], rcp[:], AF.Exp, [rcp], [rcp], scale=-1.0)
                for rc in range(2):
                    tt("dve" , olat[:, rc, hg * 512:(hg + 1) * 512], pb[rc][:, :], rcp[:], ALU.mult, [pb[rc], rcp], [olat])
            for h in range(8):
                for rc in range(2):
                    mm(pb[6 + h // 4][:, (h % 4) * 128:(h % 4 + 1) * 128], wuvb[:, rc, h, :], olat[:, rc, h * 128:(h + 1) * 128],
                       rc == 0, rc == 1, [wuvb, olat], [pb[6 + h // 4]])
            cp("act", oaT[:, :, j * 128:(j + 1) * 128], psv(6, 2).rearrange("p (a b) -> p a b", a=8), pbs(6, 2), [oaT])
        P.barrier()
        AR.free(qlatT, btat, btamt, cat, wuvs, wuvb, *pts, *tmpf, rcp, olat, maskT, maskTm, ckvT, ckvtm)

        obT = sb("obT", [8, 1024], BF16)
        qbm = [sb("qbm%d" % i, [8, 1024], BF16) for i in range(2)]
        memset("dve", qbm[0][64:128], 0.0, [qbm[0]])
        memset("dve", qbm[1][0:64], 0.0, [qbm[1]])
        uTo = sb("uTo", [16, 1024], BF16)
        dma(uTo[:].rearrange("p c t -> p (c t)"), uTo_d, [uTo_db], [uTo], "uTo_ld")
        pj = alloc_proj()
        for h in range(8):
            def ev(bs, h=h):
                for half in range(2):
                    cp("act", qbm[0][0:64, h, half * 512:(half + 1) * 512], pb[bs[half]][0:64, :], [pb[bs[half]]], [qbm[0]])
                    cp("dve", qbm[1][64:128, h, half * 512:(half + 1) * 512], pb[bs[half]][64:128, :], [pb[bs[half]]], [qbm[1]])
            proj_chunk(pj, uTo, 8 + h, ev)
        P.barrier()
        free_proj(pj)
        lamt = sb("lamt", [2, 2, 64], F32)
        lt = sb("lt", [2, 64], F32)
        lv = sb("lv", [8], F32)
        subwt = sb("subwt", [1], F32)
        cbt = sb("cbt", [8], F32)
        maddt = sb("maddt", [5, 128], F32)
        btbmt = sb("btbmt", [8, 128], F32)
        dma(lamt[:].rearrange("p a b c -> p (a b c)"), lamp, [], [lamt], "lamt")
        dma(subwt[:], subw, [], [subwt], "subwt")
        dma(cbt[:], cb, [], [cbt], "cbt")
        dma(maddt[:].rearrange("p a b -> p (a b)"), madd, [], [maddt], "maddt")
        dma(btbmt[0:16].rearrange("p a b -> p (a b)"), btbm, [], [btbmt], "btbmt")
        tt("dve", lt[:], lamt[:, :, 0, :], lamt[:, :, 1, :], ALU.mult, [lamt], [lt])
        P.add("dve", lambda e: e.tensor_reduce(out=lv[:, 0:2], in_=lt[:], axis=AX.X, op=ALU.add), reads=[lt], writes=[lv])
        act(lv[:, 2:4], lv[:, 0:2], AF.Exp, [lv], [lv])
        stt(lv[:, 4:5], lv[:, 2:3], LAM_INIT, lv[:, 3:4], ALU.add, ALU.subtract, [lv], [lv])
        ts("dve", lv[:, 5:6], lv[:, 4:5], -1.0, None, ALU.mult, None, [lv], [lv])
        ts("dve", lv[:, 6:7], subwt[:], 1.0 - LAM_INIT, None, ALU.mult, None, [subwt], [lv])
        tt("dve", btbmt[0:16], btbmt[0:16], cbt[0:16].unsqueeze(2).to_broadcast([16, 8, 128]), ALU.subtract, [btbmt, cbt], [btbmt])
        act(btbmt[0:16], btbmt[0:16], AF.Exp, [btbmt], [btbmt])
        kThs = [sb("kTh%d" % i, [4224], BF16) for i in range(2)]
        Vhs = [sb("Vh%d" % i, [33, 128], BF16) for i in range(2)]
        btbh = [sb("btbh%d" % i, [5, 128], F32) for i in range(2)]
        DB = 3
        ptb = [sb("ptb%d" % i, [512], BF16) for i in range(DB + 2)]
        om = [sb("om%d" % i, [1024], F32) for i in range(4)]
        rcpbs = [sb("rcpb%d" % i, [512], F32) for i in range(2)]
        eps128 = sb("eps128", [1], F32)
        memset("dve", eps128[:], EPS, [eps128])
        od = sb("od", [1024], F32)
        sqb = sb("sqb", [1024], BF16)
        sdb = sb("sdb", [1024], F32)
        pidx = 0
        lidx = 0
        units = []
        passidx = 0
        for h in range(8):
            for m in range(2):
                for half in range(2):
                    lo, hi = half * 512, (half + 1) * 512
                    keys = [kb for kb in range(32) if (kb // 4) * 128 < hi] + [-1]
                    for ki, kb in enumerate(keys):
                        units.append(dict(h=h, m=m, half=half, lo=lo, hi=hi, kb=kb, first=(ki == 0), last=(ki == len(keys) - 1),
                                          ab=2 * (passidx % 2), rcpb=rcpbs[passidx % 2],
                                          head_start=(m == 0 and half == 0 and ki == 0)))
                    passidx += 1

        def head_loads(h):
            kTh, Vh, bh = kThs[h % 2], Vhs[h % 2], btbh[h % 2]
            dma(kTh[:], kT_d[h], [kT_db], [kTh], kTh.name)
            dma(Vh[:], V_d[:, h * 128:(h + 1) * 128].rearrange("(kb p) d -> p kb d", p=128), [V_db], [Vh], Vh.name)
            dma(bh[:].rearrange("p a b -> p (a b)"), btb[:, h * 640:(h + 1) * 640], [], [bh], bh.name)
            stt(bh[:], bh[:], cbt[:, h:h + 1], maddt[:], ALU.subtract, ALU.add, [bh, cbt, maddt], [bh])
            act(bh[:], bh[:], AF.Exp, [bh], [bh])

        def qk_u(u):
            nonlocal pidx, lidx
            h, m, lo, hi, kb = u["h"], u["m"], u["lo"], u["hi"], u["kb"]
            if u["head_start"]:
                head_loads(h)
            kTh, bh = kThs[h % 2], btbh[h % 2]
            if kb >= 0:
                KS, c0, j0 = 128, kb * 128, kb // 4
                nears = {j0: kb % 4 + 1}
                if kb % 4 == 3 and j0 + 1 <= 7:
                    nears[j0 + 1] = 0
            else:
                KS, c0, j0 = 16, 4096, 0
                nears = {0: None}
            a = max(j0 * 128, lo)
            Lb = 4 + (lidx % 4)
            lidx += 1
            mm(pb[Lb][0:KS, a - lo:hi - lo], kTh[:, c0:c0 + KS], qbm[m][:, h, a:hi], True, True, [kTh, qbm[m]], [pb[Lb]])
            pt = ptb[pidx % (DB + 2)]
            pidx += 1
            act(pt[0:KS, a - lo:hi - lo], pb[Lb][0:KS, a - lo:hi - lo], AF.Exp, [pb[Lb]], [pt], scale=B_SCALE)
            for jn in range(a // 128, hi // 128):
                if jn not in nears:
                    break
                r = nears[jn]
                if kb >= 0:
                    bias_ap, bbuf = bh[0:KS, r, :], bh
                else:
                    bias_ap, bbuf = btbmt[0:16, h, :], btbmt
                tt("dve", pt[0:KS, jn * 128 - lo:(jn + 1) * 128 - lo], pt[0:KS, jn * 128 - lo:(jn + 1) * 128 - lo], bias_ap, ALU.mult,
                   [pt, bbuf], [pt])
            u["st"] = (pt, KS, a)

        def epi1(h):
            o0, o1 = om[(h % 2) * 2], om[(h % 2) * 2 + 1]
            stt(od[:], o1[:], lv[:, 5:6], o0[:], ALU.mult, ALU.add, [o0, o1, lv], [od])
            tt("dve", sqb[:], od[:], od[:], ALU.mult, [od], [sqb])

        def epi2(h):
            nonlocal lidx
            for half in range(2):
                Lb = 4 + (lidx % 4)
                lidx += 1
                mm(pb[Lb][:, :], onesb[:], sqb[:, half * 512:(half + 1) * 512], True, True, [onesb, sqb], [pb[Lb]])
                act(sdb[:, half * 512:(half + 1) * 512], pb[Lb][:, :], AF.Ln, [pb[Lb], eps128], [sdb], scale=1.0 / 128, bias=eps128[:])
            act(sdb[:], sdb[:], AF.Exp, [sdb], [sdb], scale=-0.5)
            stt(obT[:, h, :], od[:], lv[:, 6:7], sdb[:], ALU.mult, ALU.mult, [od, lv, sdb], [obT])

        def pv_u(u):
            h, m, half, lo, hi, kb, ab, rcpb = u["h"], u["m"], u["half"], u["lo"], u["hi"], u["kb"], u["ab"], u["rcpb"]
            pt, KS, a = u["st"]
            Vh = Vhs[h % 2]
            kbi = kb if kb >= 0 else 32
            mm(pb[ab][:, a - lo:hi - lo], Vh[0:KS, kbi, :], pt[0:KS, a - lo:hi - lo], u["first"], u["last"], [Vh, pt], [pb[ab]])
            mm(pb[ab + 1][:, a - lo:hi - lo], onesb[0:KS, :], pt[0:KS, a - lo:hi - lo], u["first"], u["last"], [onesb, pt], [pb[ab + 1]])
            if u["last"]:
                recip(rcpb[:], pb[ab + 1][:, :], [pb[ab + 1]], [rcpb])
                tt("dve", om[(h % 2) * 2 + m][:, lo:hi], pb[ab][:, :], rcpb[:], ALU.mult, [pb[ab], rcpb], [om[(h % 2) * 2 + m]])
                if h > 0 and m == 0 and half == 0:
                    epi1(h - 1)
                elif h > 0 and m == 0 and half == 1:
                    epi2(h - 1)

        nun = len(units)
        for i in range(min(DB, nun)):
            qk_u(units[i])
        for i in range(nun):
            if i + DB < nun:
                qk_u(units[i + DB])
            pv_u(units[i])
        epi1(7)
        epi2(7)
        P.barrier()
        AR.free(*qbm, lamt, lt, lv, subwt, cbt, maddt, btbmt, *kThs, *Vhs, *btbh, *ptb, *rcpbs, eps128, *om, od, sqb, sdb)

        mixT = sb("mixT", [16, 1024], BF16)
        zs = [sb("zs%d" % i, [1024], BF16) for i in range(2)]
        pj = alloc_proj()
        for br, (chunk0, oT) in enumerate(((24, oaT), (32, obT))):
            for h in range(8):
                def ev(bs, h=h, oT=oT):
                    z = zs[h % 2]
                    for half in range(2):
                        act(z[:, half * 512:(half + 1) * 512], pb[bs[half]][:, :], AF.Silu, [pb[bs[half]]], [z])
                    tt("dve", oT[:, h, :], oT[:, h, :], z[:], ALU.mult, [oT, z], [oT])
                proj_chunk(pj, uTo, chunk0 + h, ev)
        wos = [sb("wos%d" % i, [8, 128], F32) for i in range(4)]
        wobf = [sb("wobf%d" % i, [8, 128], BF16) for i in range(4)]
        sg = [sb("sg%d" % i, [1024], F32) for i in range(2)]
        t12 = [sb("t12%d" % i, [1024], F32) for i in range(2)]
        for fc in range(16):
            wa_s, wb_s = wos[(fc % 2) * 2], wos[(fc % 2) * 2 + 1]
            wa, wb = wobf[(fc % 2) * 2], wobf[(fc % 2) * 2 + 1]
            dma(wa_s[:], woa[fc * 128:(fc + 1) * 128, :].rearrange("p (c f) -> p c f", c=8), [], [wa_s], wa_s.name)
            dma2(1, wb_s[:], wob[fc * 128:(fc + 1) * 128, :].rearrange("p (c f) -> p c f", c=8), [], [wb_s], wb_s.name)
            cp("act", wa[:], wa_s[:], [wa_s], [wa])
            cp("dve", wb[:], wb_s[:], [wb_s], [wb])
            for half in range(2):
                for c in range(8):
                    mm(pb[half][:, :], wa[:, c, :], oaT[:, c, half * 512:(half + 1) * 512], c == 0, c == 7, [wa, oaT], [pb[half]])
            for half in range(2):
                for c in range(8):
                    mm(pb[2 + half][:, :], wb[:, c, :], obT[:, c, half * 512:(half + 1) * 512], c == 0, c == 7, [wb, obT], [pb[2 + half]])

            def ev_ga(bs):
                act(sg[0][:], psv(4, 2), AF.Sigmoid, pbs(4, 2), [sg[0]])
                tt("dve", t12[0][:], sg[0][:], psv(0, 2), ALU.mult, [sg[0]] + pbs(0, 2), [t12[0]])

            def ev_gb(bs, fc=fc):
                act(sg[1][:], psv(6, 2), AF.Sigmoid, pbs(6, 2), [sg[1]])
                tt("dve", t12[1][:], sg[1][:], psv(2, 2), ALU.mult, [sg[1]] + pbs(2, 2), [t12[1]])
                tt("dve", mixT[:, fc, :], t12[0][:], t12[1][:], ALU.add, [t12[0], t12[1]], [mixT])

            if fc == 0:
                pre_ga = proj_prefetch(pj, 40)
                pre_gb = proj_prefetch(pj, 56)
            nxt = {}

            def ev_ga2(bs, fc=fc):
                nxt["ga"] = proj_prefetch(pj, 40 + fc + 1) if fc + 1 < 16 else None
                ev_ga(bs)

            def ev_gb2(bs, fc=fc):
                nxt["gb"] = proj_prefetch(pj, 56 + fc + 1) if fc + 1 < 16 else None
                ev_gb(bs)

            proj_chunk(pj, uTo, 40 + fc, ev_ga2, bank_sets=((4, 5),), pre=pre_ga)
            proj_chunk(pj, uTo, 56 + fc, ev_gb2, bank_sets=((6, 7),), pre=pre_gb)
            pre_ga, pre_gb = nxt["ga"], nxt["gb"]
        P.barrier()
        free_proj(pj)
        AR.free(uTo, *zs, *wos, *wobf, *sg, *t12, oaT, obT)
        woutb = sb("woutb", [16, 2048], BF16)
        wsts = [sb("wsts%d" % i, [2048], F32) for i in range(2)]
        postwt = sb("postwt", [2048], F32)
        dma(postwt[:], postw, [], [postwt], "postwt")
        for c in range(16):
            s = wsts[c % 2]
            dma2(c, s[:], wout[c * 128:(c + 1) * 128, :], [], [s], s.name)
            cp("act" if c % 2 == 0 else "dve", woutb[:, c, :], s[:], [s], [woutb])
        xrs = [sb("xr%d" % i, [2048], F32) for i in range(2)]
        yo = [sb("yo%d" % i, [2048], F32) for i in range(2)]
        sts = [sb("st%d" % i, [16], F32) for i in range(2)]
        for j in range(8):
            b0 = 4 * (j % 2)
            st, xr, yt = sts[j % 2], xrs[j % 2], yo[j % 2]
            dma(xr[:], xq[j * 128:(j + 1) * 128, :], [], [xr], xr.name)
            for c in range(16):
                for n in range(4):
                    mm(pb[b0 + n][:, :], mixT[:, c, j * 128:(j + 1) * 128], woutb[:, c, n * 512:(n + 1) * 512], c == 0, c == 15, [mixT, woutb], [pb[b0 + n]])
            P.add("act", lambda e, st=st, b0=b0: e.activation(out=junk[:], in_=psv(b0, 4), func=AF.Square, accum_out=st[:, 0:1]), reads=pbs(b0, 4), writes=[st])
            act(st[:, 1:2], st[:, 0:1], AF.Sqrt, [st, epsb], [st], scale=1.0 / 2048, bias=epsb[:])
            recip(st[:, 2:3], st[:, 1:2], [st], [st])
            stt(yt[:], psv(b0, 4), st[:, 2:3], postwt[:], ALU.mult, ALU.mult, pbs(b0, 4) + [st, postwt], [yt])
            tt("dve", yt[:], yt[:], xr[:], ALU.add, [yt, xr], [yt])
            dmas(y[j * 128:(j + 1) * 128, :], yt[:], [yt], [], yt.name + "_st")
        P.emit(es)
    return nc


def _t5_bucket(rel):
    nb, me = 16, 8
    ret = np.where(rel > 0, nb, 0)
    n = np.abs(rel)
    nf = np.maximum(n, 1).astype(np.float32)
    large = me + (np.log(nf / np.float32(me)) / np.float32(np.log(16.0)) * np.float32(nb - me)).astype(np.int32)
    large = np.minimum(large, nb - 1)
    return ret + np.where(n < me, n, large)


_NC_CACHE = {}


def _host_inputs(inputs):
    f = lambda a: np.ascontiguousarray(np.asarray(a, dtype=np.float32))
    x = f(inputs["x"])
    meta = f(inputs["meta_tokens"])
    rel_bias = f(inputs["rel_bias"])
    w_in = f(inputs["w_in"])[0]
    o = np.cumsum([0, 1024, 256, 1024, 1024, 64, 16, 1024, 1024, 1024, 1024, 2048, 2048])
    col = lambda i: w_in[:, o[i]:o[i + 1]]
    wk = np.ascontiguousarray(np.concatenate([col(1), col(4), col(7), col(8)], axis=1))
    wq = np.concatenate([col(3), col(6), col(0), col(2), col(9), col(10), col(11)], axis=1)
    wq = np.ascontiguousarray(wq.reshape(16, 128, 72, 128).transpose(2, 1, 0, 3)).reshape(72 * 128, 2048)

    def chunk_major8(w):
        return np.ascontiguousarray(w.reshape(8, 128, 16, 128).transpose(2, 1, 0, 3)).reshape(16 * 128, 1024)
    wiw = np.ascontiguousarray(col(5))
    w_uk = f(inputs["w_uk"])[0]
    w_uv = f(inputs["w_uv"])[0]
    shared = {
        "wk": wk, "wq": wq, "wiw": wiw,
        "wukT": np.ascontiguousarray(w_uk.transpose(2, 1, 0).reshape(128, 2048)),
        "wuv": np.ascontiguousarray(w_uv.reshape(256, 1024)),
        "woa": chunk_major8(f(inputs["w_o_a"])[0]), "wob": chunk_major8(f(inputs["w_o_b"])[0]), "wout": f(inputs["w_out"])[0],
        "prewT": np.ascontiguousarray(f(inputs["pre_norm_w"])[0].reshape(16, 128).T),
        "kvw": np.ascontiguousarray(np.broadcast_to(f(inputs["kv_norm_w"])[0][None], (128, 256))),
        "ikw": np.ascontiguousarray(np.broadcast_to(np.tile(f(inputs["idx_k_norm_w"])[0], 2)[None], (128, 128))),
        "ikb": np.ascontiguousarray(np.broadcast_to(np.tile(f(inputs["idx_k_norm_b"])[0], 2)[None], (128, 128))),
        "lamp": np.ascontiguousarray(np.broadcast_to(f(inputs["diff_lambda"])[0].reshape(1, 256), (128, 256))),
        "subw": np.ascontiguousarray(f(inputs["diff_subln_w"])[0].reshape(128, 1)),
        "postw": np.ascontiguousarray(np.broadcast_to(f(inputs["post_norm_w"])[0][None], (128, 2048))),
        "ident": np.eye(128, dtype=np.float32),
        "ca": np.ascontiguousarray(np.broadcast_to(rel_bias[15, 0:8][None], (128, 8))),
        "cb": np.ascontiguousarray(np.broadcast_to(rel_bias[15, 8:16][None], (128, 8))),
    }
    s = np.arange(128)[:, None]
    t = np.arange(128)[None, :]
    in_maps = []
    for core in range(8):
        b, qq = core // 4, core % 4
        blocks = [4 * j + qq for j in range(8)]
        xk = np.zeros((4224, 2048), np.float32)
        xk[:4096] = x[b]
        xk[4096:4112] = meta
        xq = np.ascontiguousarray(x[b].reshape(32, 128, 2048)[blocks].reshape(1024, 2048))
        bta = np.zeros((128, 5, 8, 128), np.float32)
        btb = np.zeros((128, 8, 5, 128), np.float32)
        madd = np.zeros((128, 5, 128), np.float32)
        for r in range(5):
            dblk = r - 1 - qq
            rel = 128 * dblk + s - t
            bk = _t5_bucket(rel)
            bta[:, r, :, :] = rel_bias[bk][:, :, 0:8].transpose(0, 2, 1)
            btb[:, :, r, :] = rel_bias[bk][:, :, 8:16].transpose(0, 2, 1)
            allowed = ((128 * dblk + s) // 64) <= (t // 64)
            madd[:, r, :] = np.where(allowed, 0.0, NEG)
        mk = np.zeros((128, 512), np.float32)
        for rr in range(4):
            dblk = rr - qq
            tq = np.arange(128)[:, None]
            sk = np.arange(128)[None, :]
            allowed = ((128 * dblk + sk) // 64) <= (tq // 64)
            mk[:, rr * 128:(rr + 1) * 128] = np.where(allowed, 0.0, NEG)
        relm = np.arange(16)[:, None] - 16 - (128 * qq + t)
        bkm = _t5_bucket(relm)
        btam = np.ascontiguousarray(rel_bias[bkm][:, :, 0:8].transpose(0, 2, 1)).reshape(16, 1024)
        btbm = np.ascontiguousarray(rel_bias[bkm][:, :, 8:16].transpose(0, 2, 1)).reshape(16, 1024)
        m = dict(shared)
        m.update({
            "xk": xk, "xq": xq,
            "bta": np.ascontiguousarray(bta.reshape(128, 5120)), "btb": np.ascontiguousarray(btb.reshape(128, 5120)),
            "btam": btam, "btbm": btbm,
            "madd": np.ascontiguousarray(madd.reshape(128, 640)), "mk": mk,
        })
        in_maps.append(m)
    return in_maps


def kernel(**inputs):
    in_maps = _host_inputs(inputs)
    if "nc" not in _NC_CACHE:
        _NC_CACHE["nc"] = build_program()
    nc = _NC_CACHE["nc"]
    res = run_bass_kernel_spmd(nc, in_maps, core_ids=list(range(8)))
    out = np.zeros((2, 4096, 2048), np.float32)
    o4 = out.reshape(2, 32, 128, 2048)
    for core in range(8):
        b, qq = core // 4, core % 4
        yy = np.asarray(res.results[core]["y"]).reshape(8, 128, 2048)
        for j in range(8):
            o4[b, 4 * j + qq] = yy[j]
    return out
```

```python
import numpy as np
from contextlib import ExitStack
import concourse.bass as bass
import concourse.mybir as mybir
from concourse.bass_utils import run_bass_kernel_spmd

F32 = mybir.dt.float32
BF16 = mybir.dt.bfloat16
AF = mybir.ActivationFunctionType
ALU = mybir.AluOpType
AX = mybir.AxisListType

EPS = 1e-6
NEG = -30000.0
NIT = 13
A_SCALE = 128 ** -0.5
B_SCALE = 64 ** -0.5
LAM_INIT = 0.8 - 0.6 * 1.0
IW_SCALE = (16 ** -0.5) * (64 ** -0.5)
SBW = 48640


class Buf:
    __slots__ = ("name", "t", "w", "r")

    def __init__(self, name, t):
        self.name = name
        self.t = t
        self.w = {}
        self.r = {}

    def __getitem__(self, k):
        return self.t[k]


class Op:
    __slots__ = ("eng", "key", "fn", "deps", "sig", "sigval", "dma", "idx")


class Prog:
    ENG = ("pe", "act", "dve", "pool", "sp")

    def __init__(self, nc):
        self.nc = nc
        self.ops = {e: [] for e in self.ENG}
        self.latest = {}
        self.bar = {}
        self.chan_cnt = {}
        self.n = 0

    def barrier(self):
        self.bar = dict(self.latest)

    def add(self, eng, fn, reads=(), writes=(), chan=None):
        op = Op()
        op.eng = eng
        op.dma = chan is not None
        op.key = chan if chan is not None else eng
        op.fn = fn
        op.sig = op.dma
        op.sigval = None
        op.idx = self.n
        self.n += 1
        deps = {}

        def dep(o):
            if o.key == op.key and eng == "pe" and not op.dma:
                return
            p = deps.get(o.key)
            if p is None or p.idx < o.idx:
                deps[o.key] = o

        for o in self.bar.values():
            dep(o)
        for b in reads:
            for o in b.w.values():
                dep(o)
        for b in writes:
            for o in b.w.values():
                dep(o)
            for o in b.r.values():
                dep(o)
        op.deps = list(deps.values())
        for o in op.deps:
            o.sig = True
        for b in reads:
            b.r[op.key] = op
        for b in writes:
            b.w[op.key] = op
            b.r = {}
        if op.dma:
            c = self.chan_cnt.get(chan, 0) + 1
            self.chan_cnt[chan] = c
            op.sigval = 16 * c
        self.latest[op.key] = op
        self.ops[eng].append(op)
        return op

    def emit(self, es):
        nc = self.nc
        for e in self.ENG:
            c = 0
            for op in self.ops[e]:
                if op.dma:
                    continue
                if op.sig:
                    c += 1
                    op.sigval = c
        keys = set()
        for e in self.ENG:
            for op in self.ops[e]:
                if op.sig:
                    keys.add(op.key)
        sems = {}
        for k in sorted(keys):
            sems[k] = es.enter_context(nc.semaphore("s_" + k))
        block = es.enter_context(nc.Block())

        def run(e, h):
            waited = {}
            for op in self.ops[e]:
                for d in op.deps:
                    if waited.get(d.key, 0) < d.sigval:
                        h.wait_ge(sems[d.key], d.sigval)
                        waited[d.key] = d.sigval
                ins = op.fn(h)
                if op.sig:
                    ins.then_inc(sems[op.key], 16 if op.dma else 1)
            if e in ("sp", "pool"):
                for k, o in self.latest.items():
                    if o.sig and waited.get(k, 0) < o.sigval:
                        h.wait_ge(sems[k], o.sigval)
                        waited[k] = o.sigval

        @block.tensor
        def _(h):
            run("pe", h)

        @block.scalar
        def _(h):
            run("act", h)

        @block.vector
        def _(h):
            run("dve", h)

        @block.gpsimd
        def _(h):
            run("pool", h)

        @block.sync
        def _(h):
            run("sp", h)


class Arena:
    def __init__(self, ap, nwords):
        self.ap = ap
        self.n = nwords
        self.used = []

    def alloc(self, name, free_shape, dt, parts=128):
        ne = int(np.prod(free_shape))
        nw = (ne * (2 if dt == BF16 else 4) + 3) // 4
        nw = (nw + 7) // 8 * 8
        self.used.sort()
        pos = 0
        for (s, z, _) in self.used:
            if s - pos >= nw:
                break
            pos = max(pos, s + z)
        assert pos + nw <= self.n, f"SBUF arena OOM for {name}: need {nw} at {pos}, used={sum(z for _, z, _ in self.used)}"
        self.used.append((pos, nw, name))
        v = self.ap[0:parts, pos:pos + nw]
        if dt == BF16:
            v = v.bitcast(BF16)
        v = v[:, 0:ne]
        if len(free_shape) == 2:
            v = v.rearrange("p (a b) -> p a b", a=free_shape[0])
        elif len(free_shape) == 3:
            v = v.rearrange("p (a b c) -> p a b c", a=free_shape[0], b=free_shape[1])
        return Buf(name, v)

    def free(self, *bufs):
        names = {b.name for b in bufs}
        self.used = [u for u in self.used if u[2] not in names]


def build_program(dbg=()):
    nc = bass.Bass("TRN2", target_bir_lowering=False)

    def din(name, shape, dt=F32):
        return nc.dram_tensor(name, list(shape), dt, kind="ExternalInput").ap()

    xk = din("xk", [4224, 2048])
    xq = din("xq", [1024, 2048])
    wk = din("wk", [2048, 2368])
    wq = din("wq", [72 * 128, 2048])
    wiw = din("wiw", [2048, 16])
    wukT = din("wukT", [128, 2048])
    wuv = din("wuv", [256, 1024])
    woa = din("woa", [16 * 128, 1024])
    wob = din("wob", [16 * 128, 1024])
    wout = din("wout", [2048, 2048])
    prewT = din("prewT", [128, 16])
    kvw = din("kvw", [128, 256])
    ikw = din("ikw", [128, 128])
    ikb = din("ikb", [128, 128])
    lamp = din("lamp", [128, 256])
    subw = din("subw", [128, 1])
    postw = din("postw", [128, 2048])
    ident = din("ident", [128, 128])
    bta = din("bta", [128, 5 * 8 * 128])
    btb = din("btb", [128, 8 * 5 * 128])
    ca = din("ca", [128, 8])
    cb = din("cb", [128, 8])
    btam = din("btam", [16, 8 * 128])
    btbm = din("btbm", [16, 8 * 128])
    madd = din("madd", [128, 5 * 128])
    mk = din("mk", [128, 512])
    y = nc.dram_tensor("y", [1024, 2048], F32, kind="ExternalOutput").ap()
    kT_d = nc.dram_tensor("kT_d", [8, 128, 4224], BF16, kind="Internal").ap()
    V_d = nc.dram_tensor("V_d", [4224, 1024], BF16, kind="Internal").ap()
    uTo_d = nc.dram_tensor("uTo_d", [128, 16 * 1024], BF16, kind="Internal").ap()
    kT_db = Buf("kT_d", kT_d)
    V_db = Buf("V_d", V_d)
    uTo_db = Buf("uTo_d", uTo_d)
    qlat_d = nc.dram_tensor("qlat_d", [128, 2 * 8 * 1024], BF16, kind="Internal").ap()
    qlat_db = Buf("qlat_d", qlat_d)
    dbg_out = {}
    for name, shape in dbg:
        dbg_out[name] = nc.dram_tensor(name, list(shape), F32, kind="ExternalOutput").ap()

    P = Prog(nc)
    with ExitStack() as es:
        arena_ap = es.enter_context(nc.sbuf_tensor("arena", [128, SBW], F32))
        ps_ap = es.enter_context(nc.psum_tensor("psarena", [128, 4096], F32))
        AR = Arena(arena_ap, SBW)
        sb = AR.alloc
        pb = [Buf("pb%d" % i, ps_ap[:, 512 * i:512 * (i + 1)]) for i in range(8)]

        def psv(b0, nb=1):
            return ps_ap[:, 512 * b0:512 * (b0 + nb)]

        def psbf(b0, nb=1):
            return ps_ap[:, 512 * b0:512 * (b0 + nb)].bitcast(BF16)

        def pbs(b0, nb=1):
            return [pb[b0 + i] for i in range(nb)]

        def dma(out_ap, in_ap, reads, writes, chan):
            P.add("sp", lambda e: e.dma_start(out=out_ap, in_=in_ap), reads=reads, writes=writes, chan=chan)

        def dma2(q, out_ap, in_ap, reads, writes, chan):
            P.add("sp" if q % 2 == 0 else "pool", lambda e: e.dma_start(out=out_ap, in_=in_ap), reads=reads, writes=writes, chan=chan)

        def dmas(out_ap, in_ap, reads, writes, chan):
            P.add("pool", lambda e: e.dma_start(out=out_ap, in_=in_ap), reads=reads, writes=writes, chan=chan)

        def mm(out_ap, lhsT, rhs, start, stop, reads, writes):
            P.add("pe", lambda e: e.matmul(out_ap, lhsT=lhsT, rhs=rhs, start=start, stop=stop), reads=reads, writes=writes)

        def tr(out_ap, in_ap, reads, writes):
            P.add("pe", lambda e: e.transpose(out=out_ap, in_=in_ap, identity=identb[:]), reads=list(reads) + [identb], writes=writes)

        def act(out_ap, in_ap, func, reads, writes, **kw):
            P.add("act", lambda e: e.activation(out=out_ap, in_=in_ap, func=func, **kw), reads=reads, writes=writes)

        def ts(eng, out_ap, in0, s1, s2, op0, op1, reads, writes, accum_out=None):
            if accum_out is not None:
                P.add(eng, lambda e: e.tensor_scalar(out=out_ap, in0=in0, scalar1=s1, scalar2=s2, op0=op0, op1=op1, accum_out=accum_out), reads=reads, writes=writes)
            elif op1 is None:
                P.add(eng, lambda e: e.tensor_scalar(out=out_ap, in0=in0, scalar1=s1, scalar2=None, op0=op0), reads=reads, writes=writes)
            else:
                P.add(eng, lambda e: e.tensor_scalar(out=out_ap, in0=in0, scalar1=s1, scalar2=s2, op0=op0, op1=op1), reads=reads, writes=writes)

        def tt(eng, out_ap, in0, in1, op, reads, writes):
            P.add(eng, lambda e: e.tensor_tensor(out=out_ap, in0=in0, in1=in1, op=op), reads=reads, writes=writes)

        def stt(out_ap, in0, scalar, in1, op0, op1, reads, writes):
            P.add("dve", lambda e: e.scalar_tensor_tensor(out=out_ap, in0=in0, scalar=scalar, in1=in1, op0=op0, op1=op1), reads=reads, writes=writes)

        def cp(eng, out_ap, in_ap, reads, writes):
            if eng == "act":
                act(out_ap, in_ap, AF.Copy, reads, writes)
            else:
                P.add(eng, lambda e: e.tensor_copy(out=out_ap, in_=in_ap), reads=reads, writes=writes)

        def recip(out_ap, in_ap, reads, writes):
            P.add("dve", lambda e: e.reciprocal(out=out_ap, in_=in_ap), reads=reads, writes=writes)

        def memset(eng, ap, val, writes):
            P.add(eng, lambda e: e.memset(ap, val), writes=writes)

        def dbg_dump(name, src_ap, reads):
            if name in dbg_out:
                dma(dbg_out[name], src_ap, reads, [], "dbg_" + name)

        identf = sb("identf", [128], F32)
        identb = sb("identb", [128], BF16)
        onesb = sb("onesb", [128], BF16)
        prew = sb("prew", [16], F32)
        epsb = sb("epsb", [1], F32)
        junk = sb("junk", [2048], BF16)
        dma(identf[:], ident, [], [identf], "identf")
        cp("dve", identb[:], identf[:], [identf], [identb])
        memset("pool", onesb[:], 1.0, [onesb])
        memset("pool", epsb[:], EPS, [epsb])
        dma(prew[:], prewT, [], [prew], "prew")

        ikT2 = sb("ikT2", [4112], BF16)
        ckvT = sb("ckvT", [2, 4112], BF16)
        ckvtm = sb("ckvtm", [33, 256], BF16)

        def rmsnorm_a(xt, st, ub):
            P.add("act", lambda e: e.activation(out=junk[:], in_=xt[:], func=AF.Square, accum_out=st[:, 0:1]), reads=[xt], writes=[st])
            act(st[:, 1:2], st[:, 0:1], AF.Sqrt, [st, epsb], [st], scale=1.0 / 2048, bias=epsb[:])
            recip(st[:, 2:3], st[:, 1:2], [st], [st])
            ts("dve", ub[:], xt[:], st[:, 2:3], None, ALU.mult, None, [xt, st], [ub])

        def rmsnorm_b(ub, dst_ap, dst_bufs):
            for c in range(16):
                tr(psbf(0, 2)[:, c * 128:(c + 1) * 128], ub[:, c * 128:(c + 1) * 128], [ub], pbs(0, 2))
            tt("dve", dst_ap, psbf(0, 2).rearrange("p (c t) -> p c t", c=16), prew[:].unsqueeze(2).to_broadcast([128, 16, 128]),
               ALU.mult, pbs(0, 2) + [prew], dst_bufs)

        def rmsnorm_T(xt, st, ub, dst_ap, dst_bufs):
            rmsnorm_a(xt, st, ub)
            rmsnorm_b(ub, dst_ap, dst_bufs)

        wkb = sb("wkb", [16, 2368], BF16)
        wst = [sb("wst%d" % i, [1184], F32) for i in range(2)]
        kvwt = sb("kvwt", [256], F32)
        ikwt = sb("ikwt", [2, 64], F32)
        ikbt = sb("ikbt", [2, 64], F32)
        dma(kvwt[:], kvw, [], [kvwt], "kvwt")
        dma(ikwt[:], ikw.rearrange("p (a b) -> p a b", a=2), [], [ikwt], "ikwt")
        dma(ikbt[:], ikb.rearrange("p (a b) -> p a b", a=2), [], [ikbt], "ikbt")
        for c2 in range(32):
            c, hf = c2 // 2, c2 % 2
            s = wst[c2 % 2]
            dma2(c2, s[:], wk[c * 128:(c + 1) * 128, hf * 1184:(hf + 1) * 1184], [], [s], s.name)
            cp("act" if c2 % 2 == 0 else "dve", wkb[:, c, hf * 1184:(hf + 1) * 1184], s[:], [s], [wkb])
        xts = [sb("xt%d" % i, [2048], F32) for i in range(2)]
        ubs = [sb("ub%d" % i, [2048], BF16) for i in range(2)]
        uTs = [sb("uT%d" % i, [16, 128], BF16) for i in range(2)]
        sts = [sb("st%d" % i, [16], F32) for i in range(2)]
        st2s = [sb("st2%d" % i, [16], F32) for i in range(2)]
        ckik = [sb("ckik%d" % i, [320], F32) for i in range(2)]
        kbtm = [sb("kbtm%d" % i, [1024], BF16) for i in range(2)]
        kTblk = [sb("kTblk%d" % i, [8, 128], BF16) for i in range(2)]
        vtm = [sb("vtm%d" % i, [1024], BF16) for i in range(2)]
        ikc = sb("ikc", [64], F32)
        ikdf = sb("ikdf", [2, 64], F32)
        ikds = [sb("ikd%d" % i, [2, 64], BF16) for i in range(2)]
        GRP = [(0, 320), (320, 512), (832, 512), (1344, 512), (1856, 512)]

        def front0a(tb):
            xt, ub, st = xts[tb % 2], ubs[tb % 2], sts[tb % 2]
            dma(xt[:], xk[tb * 128:(tb + 1) * 128, :], [], [xt], xt.name)
            rmsnorm_a(xt, st, ub)

        def front0b(tb):
            rmsnorm_b(ubs[tb % 2], uTs[tb % 2][:], [uTs[tb % 2]])

        def post0(tb):
            nv = 128 if tb < 32 else 16
            col0 = tb * 128
            ikd, kb_, kt = ikds[tb % 2], kbtm[tb % 2], kTblk[tb % 2]
            pk = psbf(7)
            for rc in range(2):
                tr(pk[:, 512 + rc * 128:512 + (rc + 1) * 128], ckvtm[:, tb, rc * 128:(rc + 1) * 128], [ckvtm], [pb[7]])
            tr(pk[:, 768:896], ikd[:].rearrange("p a b -> p (a b)"), [ikd], [pb[7]])
            cp("act", ckvT[:, :, col0:col0 + nv], pk[:, 512:768].rearrange("p (a b) -> p a b", a=2)[:, :, 0:nv], [pb[7]], [ckvT])
            cp("act", ikT2[:, col0:col0 + nv], pk[:, 768:768 + nv], [pb[7]], [ikT2])
            for half in range(2):
                for q in range(4):
                    hh = half * 4 + q
                    tr(pk[:, q * 128:(q + 1) * 128], kb_[:, hh * 128:(hh + 1) * 128], [kb_], [pb[7]])
                cp("dve" if half == 0 else "act", kt[:, half * 4:(half + 1) * 4, :], pk[:, 0:512].rearrange("p (a b) -> p a b", a=4), [pb[7]], [kt])
            dmas(kT_d.rearrange("h p n -> p h n")[:, :, col0:col0 + 128], kt[:], [kt], [kT_db], kt.name)

        front0a(0)
        front0b(0)
        for tb in range(33):
            uT, st, ck, kb_, vt, ikd = uTs[tb % 2], st2s[tb % 2], ckik[tb % 2], kbtm[tb % 2], vtm[tb % 2], ikds[tb % 2]
            col0 = tb * 128
            if tb + 1 < 33:
                front0a(tb + 1)
            for c in range(16):
                if c == 8 and tb + 1 < 33:
                    front0b(tb + 1)
                for n, (n0, w) in enumerate(GRP):
                    mm(pb[2 + n][:, 0:w], uT[:, c, :], wkb[:, c, n0:n0 + w], c == 0, c == 15, [uT, wkb], [pb[2 + n]])
            cp("act", ck[:], pb[2][:, 0:320], [pb[2]], [ck])
            cp("act", kb_[:], psv(3, 2), pbs(3, 2), [kb_])
            cp("dve", vt[:], psv(5, 2), pbs(5, 2), [vt])
            dmas(V_d[col0:col0 + 128, :], vt[:], [vt], [V_db], vt.name)
            if tb > 0:
                post0(tb - 1)
            P.add("act", lambda e, st=st, ck=ck: e.activation(out=junk[:, 0:256], in_=ck[:, 0:256], func=AF.Square, accum_out=st[:, 3:4]), reads=[ck], writes=[st])
            act(st[:, 4:5], st[:, 3:4], AF.Sqrt, [st, epsb], [st], scale=1.0 / 256, bias=epsb[:])
            P.add("act", lambda e, st=st, ck=ck: e.activation(out=junk[:, 256:320], in_=ck[:, 256:320], func=AF.Copy, accum_out=st[:, 6:7]), reads=[ck], writes=[st])
            P.add("act", lambda e, st=st, ck=ck: e.activation(out=junk[:, 320:384], in_=ck[:, 256:320], func=AF.Square, accum_out=st[:, 7:8]), reads=[ck], writes=[st])
            recip(st[:, 5:6], st[:, 4:5], [st], [st])
            stt(ckvtm[:, tb, :], ck[:, 0:256], st[:, 5:6], kvwt[:], ALU.mult, ALU.mult, [ck, st, kvwt], [ckvtm])
            ts("dve", st[:, 8:9], st[:, 6:7], 1.0 / 64, None, ALU.mult, None, [st], [st])
            tt("dve", st[:, 9:10], st[:, 8:9], st[:, 8:9], ALU.mult, [st], [st])
            stt(st[:, 10:11], st[:, 7:8], 1.0 / 64, st[:, 9:10], ALU.mult, ALU.subtract, [st], [st])
            act(st[:, 11:12], st[:, 10:11], AF.Sqrt, [st, epsb], [st], scale=1.0, bias=epsb[:])
            recip(st[:, 12:13], st[:, 11:12], [st], [st])
            ts("dve", ikc[:], ck[:, 256:320], st[:, 8:9], st[:, 12:13], ALU.subtract, ALU.mult, [ck, st], [ikc])
            tt("dve", ikdf[:], ikc[:].unsqueeze(1).to_broadcast([128, 2, 64]), ikwt[:], ALU.mult, [ikc, ikwt], [ikdf])
            tt("dve", ikd[:], ikdf[:], ikbt[:], ALU.add, [ikdf, ikbt], [ikd])
        post0(32)
        P.barrier()
        AR.free(wkb, *wst, kvwt, ikwt, ikbt, *xts, *ubs, *uTs, *sts, *st2s, *ckik, *kbtm, *kTblk, *vtm, ikc, ikdf, *ikds)
        if "d_ckv" in dbg_out:
            tmpd = sb("tmpd", [33, 256], F32)
            cp("dve", tmpd[:], ckvtm[:], [ckvtm], [tmpd])
            dma(dbg_out["d_ckv"], tmpd[:], [tmpd], [], "dbg1")
            tmpe = sb("tmpe", [4112], F32)
            cp("dve", tmpe[:], ikT2[:], [ikT2], [tmpe])
            dma(dbg_out["d_ikT"], tmpe[:], [tmpe], [], "dbg2")
            tmpf_ = sb("tmpf_", [2, 4112], F32)
            cp("dve", tmpf_[:], ckvT[:], [ckvT], [tmpf_])
            dma(dbg_out["d_ckvT"], tmpf_[:], [tmpf_], [], "dbg3")
        if "stop0" in dbg_out:
            P.emit(es)
            return nc

        def alloc_proj():
            d = {}
            d["wstc"] = [sb("wstc%d" % i, [16, 128], F32) for i in range(3)]
            d["wbfc"] = [sb("wbfc%d" % i, [16, 128], BF16) for i in range(2)]
            d["k"] = 0
            return d

        def free_proj(d):
            AR.free(*d["wstc"], *d["wbfc"])

        def proj_prefetch(d, chunk):
            k = d["k"]
            d["k"] += 1
            s, w = d["wstc"][k % 3], d["wbfc"][k % 2]
            dma2(k, s[:], wq[chunk * 128:(chunk + 1) * 128, :].rearrange("p (c f) -> p c f", c=16), [], [s], s.name)
            cp("act" if k % 2 == 0 else "dve", w[:], s[:], [s], [w])
            return (k, w)

        def proj_chunk(d, uT, chunk, evac, bank_sets=((0, 1), (2, 3)), pre=None):
            k, w = pre if pre is not None else proj_prefetch(d, chunk)
            bs = bank_sets[k % len(bank_sets)]
            for half in range(2):
                for c in range(16):
                    mm(pb[bs[half]][:, :], w[:, c, :], uT[:, c, half * 512:(half + 1) * 512], c == 0, c == 15, [w, uT], [pb[bs[half]]])
            evac(bs)

        iqm = [sb("iqm%d" % i, [8, 1024], BF16) for i in range(2)]
        memset("dve", iqm[0][64:128], 0.0, [iqm[0]])
        memset("dve", iqm[1][0:64], 0.0, [iqm[1]])
        iwS = sb("iwS", [8, 16], F32)
        uTo = sb("uTo", [16, 1024], BF16)
        qlatT = sb("qlatT", [2, 8, 1024], BF16)
        xts = [sb("xt%d" % i, [2048], F32) for i in range(2)]
        ubs = [sb("ub%d" % i, [2048], BF16) for i in range(2)]
        sts = [sb("st%d" % i, [16], F32) for i in range(2)]
        wiws = sb("wiws", [16, 16], F32)
        wiwb = sb("wiwb", [16, 16], BF16)
        P.add("sp", lambda e: e.dma_start(out=wiws[:], in_=wiw.rearrange("(c p) f -> p c f", p=128)), reads=[], writes=[wiws], chan="wiws")
        cp("dve", wiwb[:], wiws[:], [wiws], [wiwb])
        for j in range(8):
            xt, ub, st = xts[j % 2], ubs[j % 2], sts[j % 2]
            dma(xt[:], xq[j * 128:(j + 1) * 128, :], [], [xt], xt.name)
            rmsnorm_T(xt, st, ub, uTo[:, :, j * 128:(j + 1) * 128], [uTo])
        dmas(uTo_d, uTo[:].rearrange("p c t -> p (c t)"), [uTo], [uTo_db], "uTo_st")
        for j in range(8):
            for c in range(16):
                mm(pb[7][:, j * 16:(j + 1) * 16], uTo[:, c, j * 128:(j + 1) * 128], wiwb[:, c, :], c == 0, c == 15, [uTo, wiwb], [pb[7]])
        act(iwS[:], pb[7][:, 0:128].rearrange("p (a b) -> p a b", a=8), AF.Copy, [pb[7]], [iwS], scale=IW_SCALE)
        P.barrier()
        AR.free(*xts, *ubs, *sts, wiws, wiwb)
        pj = alloc_proj()
        for hp in range(8):
            def ev(bs, hp=hp):
                for half in range(2):
                    cp("act", iqm[0][0:64, hp, half * 512:(half + 1) * 512], pb[bs[half]][0:64, :], [pb[bs[half]]], [iqm[0]])
                    cp("dve", iqm[1][64:128, hp, half * 512:(half + 1) * 512], pb[bs[half]][64:128, :], [pb[bs[half]]], [iqm[1]])
            proj_chunk(pj, uTo, hp, ev)
        wukb = sb("wukb", [8, 256], BF16)
        wuks = pj["wstc"][2]
        dma(wuks[:].rearrange("p a b -> p (a b)"), wukT, [], [wuks], wuks.name)
        cp("dve", wukb[:], wuks[:].rearrange("p a b -> p (a b)").rearrange("p (h r) -> p h r", h=8), [wuks], [wukb])
        qaT = [sb("qaT%d" % i, [1024], BF16) for i in range(2)]
        pend_ql = []
        for h in range(8):
            def ev(bs, h=h):
                q = qaT[h % 2]
                for half in range(2):
                    cp("act" if half == 0 else "dve", q[:, half * 512:(half + 1) * 512], pb[bs[half]][:, :], [pb[bs[half]]], [q])

                def ql():
                    for rc in range(2):
                        for half in range(2):
                            bk = 4 + rc * 2 + half
                            mm(pb[bk][:, :], wukb[:, h, rc * 128:(rc + 1) * 128], q[:, half * 512:(half + 1) * 512], True, True, [wukb, q], [pb[bk]])
                        cp("act" if rc == 0 else "dve", qlatT[:, rc, h, :], psv(4 + rc * 2, 2), pbs(4 + rc * 2, 2), [qlatT])
                pend_ql.append(ql)
            proj_chunk(pj, uTo, 16 + h, ev)
            if len(pend_ql) > 1:
                pend_ql.pop(0)()
        while pend_ql:
            pend_ql.pop(0)()
        dmas(qlat_d, qlatT[:].rearrange("p a b c -> p (a b c)"), [qlatT], [qlat_db], "qlat_st")
        P.barrier()
        free_proj(pj)
        AR.free(uTo, qlatT, wukb, *qaT)

        maskT = sb("maskT", [144, 128], BF16)
        maskTm = sb("maskTm", [8, 128], BF16)
        score = [sb("score%d" % i, [16 + 4096], F32) for i in range(2)]
        rb = [sb("rb%d" % i, [512], BF16) for i in range(4)]
        diag = [sb("diag%d" % i, [16, 128], BF16) for i in range(2)]
        selb = [sb("selb%d" % i, [16 + 4096], BF16) for i in range(2)]
        mkt = sb("mkt", [512], F32)
        tbs = [sb("tb%d" % i, [8], F32) for i in range(2)]
        dma(mkt[:], mk, [], [mkt], "mkt")
        sidx = 0
        gidx = 0

        def scoring_units(j):
            units = []
            dg = diag[j % 2]
            sc = score[j % 2]

            def u0():
                tt("dve", dg[:], identb[:].unsqueeze(1).to_broadcast([128, 16, 128]),
                   iwS[:, j, :].unsqueeze(2).to_broadcast([128, 16, 128]), ALU.mult, [identb, iwS], [dg])
            units.append(u0)
            for g in range(j + 2):
                def ug(g=g):
                    nonlocal sidx, gidx
                    if g <= j:
                        c0, w, d0 = g * 512, 512, 16 + g * 512
                    else:
                        c0, w, d0 = 4096, 16, 0
                    scb = 4 + (gidx % 2)
                    gidx += 1
                    pend = []

                    def qk(h):
                        nonlocal sidx
                        hp, hh = h // 2, h % 2
                        bk = sidx % 4
                        r = rb[sidx % 4]
                        sidx += 1
                        mm(pb[bk][:, 0:w], iqm[hh][:, hp, j * 128:(j + 1) * 128],
                           ikT2[:, c0:c0 + w], True, True, [iqm[hh], ikT2], [pb[bk]])
                        act(r[:, 0:w], pb[bk][:, 0:w], AF.Relu, [pb[bk]], [r])
                        pend.append((h, r))

                    def dgm():
                        h, r = pend.pop(0)
                        mm(pb[scb][:, 0:w], dg[:, h, :], r[:, 0:w], h == 0, h == 15, [dg, r], [pb[scb]])

                    for h in range(16):
                        qk(h)
                        if h >= 2:
                            dgm()
                    dgm()
                    dgm()
                    if g == j:
                        tt("dve", sc[:, d0:d0 + w], pb[scb][:, 0:w], mkt[:], ALU.add, [pb[scb], mkt], [sc])
                    else:
                        cp("act", sc[:, d0:d0 + w], pb[scb][:, 0:w], [pb[scb]], [sc])
                units.append(ug)
            return units

        def bisect_units(j):
            units = []
            sc = score[j % 2]
            n = 16 + (j + 1) * 512
            tbv = tbs[j % 2]
            sl = selb[j % 2]
            units.append(lambda: memset("dve", tbv[:, 0:1], 0.0, [tbv]))
            for k in range(NIT):
                def uk(k=k):
                    step = 3.0 / (2 ** k)
                    P.add("dve", lambda e: e.tensor_scalar(
                        out=sl[:, 0:n], in0=sc[:, 0:n], scalar1=tbv[:, 0:1], scalar2=None, op0=ALU.is_gt, op1=ALU.add,
                        accum_out=tbv[:, 1:2]), reads=[sc, tbv], writes=[tbv, sl])
                    ts("dve", tbv[:, 2:3], tbv[:, 1:2], 255.5, 2.0 * step, ALU.is_ge, ALU.mult, [tbv], [tbv])
                    stt(tbv[:, 0:1], tbv[:, 2:3], -step, tbv[:, 0:1], ALU.add, ALU.add, [tbv], [tbv])
                units.append(uk)

            def ufin():
                ts("dve", tbv[:, 3:4], tbv[:, 0:1], -3.0 / (2 ** (NIT - 1)), None, ALU.add, None, [tbv], [tbv])
                ts("dve", sl[:, 0:n], sc[:, 0:n], tbv[:, 3:4], None, ALU.is_gt, None, [sc, tbv], [sl])
                if j == 1 and "d_score" in dbg_out:
                    dma(dbg_out["d_score"], sc[:, 0:16 + 1024], [sc], [], "dbgs")
                    dma(dbg_out["d_thr"], tbv[:], [tbv], [], "dbgt")
                base = 2 * j * j + 2 * j
                nkb = 4 * j + 4
                tr(psbf(6)[0:16, 0:128], sl[:, 0:16], [sl], [pb[6]])
                cp("act", maskTm[0:16, j, :], psbf(6)[0:16, 0:128], [pb[6]], [maskTm])
                for k0 in range(0, nkb, 8):
                    kn = min(8, nkb - k0)
                    bk = 6 + ((k0 // 8) % 2)
                    for q in range(kn):
                        kb = k0 + q
                        tr(psbf(bk)[:, q * 128:(q + 1) * 128], sl[:, 16 + kb * 128:16 + (kb + 1) * 128], [sl], [pb[bk]])
                    cp("act", maskT[:, base + k0:base + k0 + kn, :],
                       psbf(bk)[:, 0:kn * 128].rearrange("p (a b) -> p a b", a=kn), [pb[bk]], [maskT])
            units.append(ufin)
            return units

        prev = []
        for jj in range(9):
            j = 7 - jj
            su = scoring_units(j) if jj < 8 else []
            bu = prev
            if su:
                per = -(-len(bu) // len(su))
                for u in su:
                    u()
                    for _ in range(per):
                        if bu:
                            bu.pop(0)()
            while bu:
                bu.pop(0)()
            prev = bisect_units(j) if jj < 8 else []
        P.barrier()
        AR.free(*score, *rb, *diag, *selb, mkt, *tbs, *iqm, iwS, ikT2)
        if "stop2" in dbg_out:
            P.emit(es)
            return nc

        oaT = sb("oaT", [8, 1024], BF16)
        qlatT = sb("qlatT", [2, 8, 1024], BF16)
        for q4 in range(4):
            dma2(q4, qlatT[:].rearrange("p a b c -> p (a b c)")[:, q4 * 4096:(q4 + 1) * 4096], qlat_d[:, q4 * 4096:(q4 + 1) * 4096],
                 [qlat_db], [qlatT], "qlat_ld%d" % q4)
        btat = sb("btat", [5, 8, 128], F32)
        btamt = sb("btamt", [8, 128], F32)
        cat = sb("cat", [8], F32)
        wuvs = sb("wuvs", [2, 1024], F32)
        wuvb = sb("wuvb", [2, 8, 128], BF16)
        dma(btat[:].rearrange("p a b c -> p (a b c)"), bta, [], [btat], "btat")
        dma(btamt[0:16].rearrange("p a b -> p (a b)"), btam, [], [btamt], "btamt")
        dma(cat[:], ca, [], [cat], "cat")
        dma(wuvs[:], wuv.rearrange("(rc p) n -> p rc n", p=128), [], [wuvs], "wuvs")
        cp("dve", wuvb[:].rearrange("p a b c -> p a (b c)"), wuvs[:], [wuvs], [wuvb])
        for r in range(5):
            tt("dve", btat[:, r, :, :], btat[:, r, :, :], cat[:].unsqueeze(2).to_broadcast([128, 8, 128]), ALU.subtract, [btat, cat], [btat])
        tt("dve", btamt[0:16], btamt[0:16], cat[0:16].unsqueeze(2).to_broadcast([16, 8, 128]), ALU.subtract, [btamt, cat], [btamt])
        act(btat[:], btat[:], AF.Exp, [btat], [btat])
        act(btamt[0:16], btamt[0:16], AF.Exp, [btamt], [btamt])
        DA = 3
        pts = [sb("pt%d" % i, [4, 128], BF16) for i in range(DA + 2)]
        tmpf = [sb("tmpf%d" % i, [4, 128], BF16) for i in range(3)]
        rcp = sb("rcp", [512], F32)
        olat = sb("olat", [2, 1024], BF16)
        uidx = 0
        tfidx = 0
        LB_A = [3, 4, 5, 6, 7]
        lrot = 0
        for j in range(8):
            nkb = 4 * j + 4
            base = 2 * j * j + 2 * j
            for hg in range(2):
                units = list(range(nkb)) + [-1]
                nun = len(units)
                state = {}

                def qk_unit(u):
                    nonlocal uidx, tfidx, lrot
                    kb = units[u]
                    if kb >= 0:
                        KS, c0 = 128, kb * 128
                        rr = kb - (4 * j - 1)
                        near = btat[:, rr, hg * 4:(hg + 1) * 4, :] if rr >= 0 else None
                        mT = maskT[:, base + kb, :]
                        mbuf = maskT
                    else:
                        KS, c0 = 16, 4096
                        near = btamt[0:16, hg * 4:(hg + 1) * 4, :] if j == 0 else None
                        mT = maskTm[0:16, j, :]
                        mbuf = maskTm
                    Lb = LB_A[lrot % 5]
                    lrot += 1
                    for rc in range(2):
                        mm(pb[Lb][0:KS, :].rearrange("p (a b) -> p a b", a=4), ckvT[:, rc, c0:c0 + KS], qlatT[:, rc, hg * 4:(hg + 1) * 4, j * 128:(j + 1) * 128],
                           rc == 0, rc == 1, [ckvT, qlatT], [pb[Lb]])
                    pt = pts[uidx % (DA + 2)]
                    uidx += 1
                    Lv = pb[Lb][0:KS, :].rearrange("p (a b) -> p a b", a=4)
                    act(pt[0:KS], Lv, AF.Exp, [pb[Lb]], [pt], scale=A_SCALE)
                    if near is not None:
                        tf = tmpf[tfidx % 3]
                        tfidx += 1
                        tt("dve", tf[0:KS], near, mT.unsqueeze(1).to_broadcast([KS, 4, 128]), ALU.mult, [btat, btamt, mbuf], [tf])
                        tt("dve", pt[0:KS], pt[0:KS], tf[0:KS], ALU.mult, [pt, tf], [pt])
                    else:
                        tt("dve", pt[0:KS], pt[0:KS], mT.unsqueeze(1).to_broadcast([KS, 4, 128]), ALU.mult, [pt, mbuf], [pt])
                    state[u] = (pt, KS, kb)

                def pv_unit(u):
                    pt, KS, kb = state.pop(u)
                    kbi = kb if kb >= 0 else 32
                    first = u == 0
                    last = u == nun - 1
                    rhs = pt[0:KS].rearrange("p a b -> p (a b)")
                    for rc in range(2):
                        mm(pb[rc][:, :], ckvtm[0:KS, kbi, rc * 128:(rc + 1) * 128], rhs, first, last, [ckvtm, pt], [pb[rc]])
                    mm(pb[2][:, :], onesb[0:KS, :], rhs, first, last, [onesb, pt], [pb[2]])

                for u in range(min(DA, nun)):
                    qk_unit(u)
                for u in range(nun):
                    if u + DA < nun:
                        qk_unit(u + DA)
                    pv_unit(u)
                act(rcp[:], pb[2][:, :], AF.Ln, [pb[2]], [rcp])
                act(rcp[:], rcp[:], AF.Exp, [rcp], [rcp], scale=-1.0)
                for rc in range(2):
                    tt("dve" , olat[:, rc, hg * 512:(hg + 1) * 512], pb[rc][:, :], rcp[:], ALU.mult, [pb[rc], rcp], [olat])
            for h in range(8):
                for rc in range(2):
                    mm(pb[6 + h // 4][:, (h % 4) * 128:(h % 4 + 1) * 128], wuvb[:, rc, h, :], olat[:, rc, h * 128:(h + 1) * 128],
                       rc == 0, rc == 1, [wuvb, olat], [pb[6 + h // 4]])
            cp("act", oaT[:, :, j * 128:(j + 1) * 128], psv(6, 2).rearrange("p (a b) -> p a b", a=8), pbs(6, 2), [oaT])
        P.barrier()
        AR.free(qlatT, btat, btamt, cat, wuvs, wuvb, *pts, *tmpf, rcp, olat, maskT, maskTm, ckvT, ckvtm)

        obT = sb("obT", [8, 1024], BF16)
        qbm = [sb("qbm%d" % i, [8, 1024], BF16) for i in range(2)]
        memset("dve", qbm[0][64:128], 0.0, [qbm[0]])
        memset("dve", qbm[1][0:64], 0.0, [qbm[1]])
        uTo = sb("uTo", [16, 1024], BF16)
        for q4 in range(4):
            dma2(q4, uTo[:].rearrange("p c t -> p (c t)")[:, q4 * 4096:(q4 + 1) * 4096], uTo_d[:, q4 * 4096:(q4 + 1) * 4096],
                 [uTo_db], [uTo], "uTo_ld%d" % q4)
        pj = alloc_proj()
        for h in range(8):
            def ev(bs, h=h):
                for half in range(2):
                    cp("act", qbm[0][0:64, h, half * 512:(half + 1) * 512], pb[bs[half]][0:64, :], [pb[bs[half]]], [qbm[0]])
                    cp("dve", qbm[1][64:128, h, half * 512:(half + 1) * 512], pb[bs[half]][64:128, :], [pb[bs[half]]], [qbm[1]])
            proj_chunk(pj, uTo, 8 + h, ev)
        P.barrier()
        free_proj(pj)
        lamt = sb("lamt", [2, 2, 64], F32)
        lt = sb("lt", [2, 64], F32)
        lv = sb("lv", [8], F32)
        subwt = sb("subwt", [1], F32)
        cbt = sb("cbt", [8], F32)
        maddt = sb("maddt", [5, 128], F32)
        btbmt = sb("btbmt", [8, 128], F32)
        dma(lamt[:].rearrange("p a b c -> p (a b c)"), lamp, [], [lamt], "lamt")
        dma(subwt[:], subw, [], [subwt], "subwt")
        dma(cbt[:], cb, [], [cbt], "cbt")
        dma(maddt[:].rearrange("p a b -> p (a b)"), madd, [], [maddt], "maddt")
        dma(btbmt[0:16].rearrange("p a b -> p (a b)"), btbm, [], [btbmt], "btbmt")
        tt("dve", lt[:], lamt[:, :, 0, :], lamt[:, :, 1, :], ALU.mult, [lamt], [lt])
        P.add("dve", lambda e: e.tensor_reduce(out=lv[:, 0:2], in_=lt[:], axis=AX.X, op=ALU.add), reads=[lt], writes=[lv])
        act(lv[:, 2:4], lv[:, 0:2], AF.Exp, [lv], [lv])
        stt(lv[:, 4:5], lv[:, 2:3], LAM_INIT, lv[:, 3:4], ALU.add, ALU.subtract, [lv], [lv])
        ts("dve", lv[:, 5:6], lv[:, 4:5], -1.0, None, ALU.mult, None, [lv], [lv])
        ts("dve", lv[:, 6:7], subwt[:], 1.0 - LAM_INIT, None, ALU.mult, None, [subwt], [lv])
        tt("dve", btbmt[0:16], btbmt[0:16], cbt[0:16].unsqueeze(2).to_broadcast([16, 8, 128]), ALU.subtract, [btbmt, cbt], [btbmt])
        act(btbmt[0:16], btbmt[0:16], AF.Exp, [btbmt], [btbmt])
        kThs = [sb("kTh%d" % i, [4224], BF16) for i in range(2)]
        Vhs = [sb("Vh%d" % i, [33, 128], BF16) for i in range(2)]
        btbh = [sb("btbh%d" % i, [5, 128], F32) for i in range(2)]
        DB = 3
        ptb = [sb("ptb%d" % i, [512], BF16) for i in range(DB + 2)]
        om = [sb("om%d" % i, [1024], F32) for i in range(4)]
        rcpbs = [sb("rcpb%d" % i, [512], F32) for i in range(2)]
        eps128 = sb("eps128", [1], F32)
        memset("dve", eps128[:], EPS, [eps128])
        od = sb("od", [1024], F32)
        sqb = sb("sqb", [1024], BF16)
        sdb = sb("sdb", [1024], F32)
        pidx = 0
        lidx = 0
        units = []
        passidx = 0
        for h in range(8):
            for m in range(2):
                for half in range(2):
                    lo, hi = half * 512, (half + 1) * 512
                    keys = [kb for kb in range(32) if (kb // 4) * 128 < hi] + [-1]
                    for ki, kb in enumerate(keys):
                        units.append(dict(h=h, m=m, half=half, lo=lo, hi=hi, kb=kb, first=(ki == 0), last=(ki == len(keys) - 1),
                                          ab=2 * (passidx % 2), rcpb=rcpbs[passidx % 2],
                                          head_start=(m == 0 and half == 0 and ki == 0)))
                    passidx += 1

        def head_loads(h):
            kTh, Vh, bh = kThs[h % 2], Vhs[h % 2], btbh[h % 2]
            dma(kTh[:], kT_d[h], [kT_db], [kTh], kTh.name)
            dma(Vh[:], V_d[:, h * 128:(h + 1) * 128].rearrange("(kb p) d -> p kb d", p=128), [V_db], [Vh], Vh.name)
            dma(bh[:].rearrange("p a b -> p (a b)"), btb[:, h * 640:(h + 1) * 640], [], [bh], bh.name)
            stt(bh[:], bh[:], cbt[:, h:h + 1], maddt[:], ALU.subtract, ALU.add, [bh, cbt, maddt], [bh])
            act(bh[:], bh[:], AF.Exp, [bh], [bh])

        def qk_u(u):
            nonlocal pidx, lidx
            h, m, lo, hi, kb = u["h"], u["m"], u["lo"], u["hi"], u["kb"]
            if u["head_start"]:
                head_loads(h)
            kTh, bh = kThs[h % 2], btbh[h % 2]
            if kb >= 0:
                KS, c0, j0 = 128, kb * 128, kb // 4
                nears = {j0: kb % 4 + 1}
                if kb % 4 == 3 and j0 + 1 <= 7:
                    nears[j0 + 1] = 0
            else:
                KS, c0, j0 = 16, 4096, 0
                nears = {0: None}
            a = max(j0 * 128, lo)
            Lb = 4 + (lidx % 4)
            lidx += 1
            mm(pb[Lb][0:KS, a - lo:hi - lo], kTh[:, c0:c0 + KS], qbm[m][:, h, a:hi], True, True, [kTh, qbm[m]], [pb[Lb]])
            pt = ptb[pidx % (DB + 2)]
            pidx += 1
            act(pt[0:KS, a - lo:hi - lo], pb[Lb][0:KS, a - lo:hi - lo], AF.Exp, [pb[Lb]], [pt], scale=B_SCALE)
            for jn in range(a // 128, hi // 128):
                if jn not in nears:
                    break
                r = nears[jn]
                if kb >= 0:
                    bias_ap, bbuf = bh[0:KS, r, :], bh
                else:
                    bias_ap, bbuf = btbmt[0:16, h, :], btbmt
                tt("dve", pt[0:KS, jn * 128 - lo:(jn + 1) * 128 - lo], pt[0:KS, jn * 128 - lo:(jn + 1) * 128 - lo], bias_ap, ALU.mult,
                   [pt, bbuf], [pt])
            u["st"] = (pt, KS, a)

        def epi1(h):
            o0, o1 = om[(h % 2) * 2], om[(h % 2) * 2 + 1]
            stt(od[:], o1[:], lv[:, 5:6], o0[:], ALU.mult, ALU.add, [o0, o1, lv], [od])
            tt("dve", sqb[:], od[:], od[:], ALU.mult, [od], [sqb])

        def epi2(h):
            nonlocal lidx
            for half in range(2):
                Lb = 4 + (lidx % 4)
                lidx += 1
                mm(pb[Lb][:, :], onesb[:], sqb[:, half * 512:(half + 1) * 512], True, True, [onesb, sqb], [pb[Lb]])
                act(sdb[:, half * 512:(half + 1) * 512], pb[Lb][:, :], AF.Ln, [pb[Lb], eps128], [sdb], scale=1.0 / 128, bias=eps128[:])
            act(sdb[:], sdb[:], AF.Exp, [sdb], [sdb], scale=-0.5)
            stt(obT[:, h, :], od[:], lv[:, 6:7], sdb[:], ALU.mult, ALU.mult, [od, lv, sdb], [obT])

        def pv_u(u):
            h, m, half, lo, hi, kb, ab, rcpb = u["h"], u["m"], u["half"], u["lo"], u["hi"], u["kb"], u["ab"], u["rcpb"]
            pt, KS, a = u["st"]
            Vh = Vhs[h % 2]
            kbi = kb if kb >= 0 else 32
            mm(pb[ab][:, a - lo:hi - lo], Vh[0:KS, kbi, :], pt[0:KS, a - lo:hi - lo], u["first"], u["last"], [Vh, pt], [pb[ab]])
            mm(pb[ab + 1][:, a - lo:hi - lo], onesb[0:KS, :], pt[0:KS, a - lo:hi - lo], u["first"], u["last"], [onesb, pt], [pb[ab + 1]])
            if u["last"]:
                recip(rcpb[:], pb[ab + 1][:, :], [pb[ab + 1]], [rcpb])
                tt("dve", om[(h % 2) * 2 + m][:, lo:hi], pb[ab][:, :], rcpb[:], ALU.mult, [pb[ab], rcpb], [om[(h % 2) * 2 + m]])
                if h > 0 and m == 0 and half == 0:
                    epi1(h - 1)
                elif h > 0 and m == 0 and half == 1:
                    epi2(h - 1)

        nun = len(units)
        for i in range(min(DB, nun)):
            qk_u(units[i])
        for i in range(nun):
            if i + DB < nun:
                qk_u(units[i + DB])
            pv_u(units[i])
        epi1(7)
        epi2(7)
        P.barrier()
        AR.free(*qbm, lamt, lt, lv, subwt, cbt, maddt, btbmt, *kThs, *Vhs, *btbh, *ptb, *rcpbs, eps128, *om, od, sqb, sdb)

        mixT = sb("mixT", [16, 1024], BF16)
        zs = [sb("zs%d" % i, [1024], BF16) for i in range(2)]
        pj = alloc_proj()
        for br, (chunk0, oT) in enumerate(((24, oaT), (32, obT))):
            for h in range(8):
                def ev(bs, h=h, oT=oT):
                    z = zs[h % 2]
                    for half in range(2):
                        act(z[:, half * 512:(half + 1) * 512], pb[bs[half]][:, :], AF.Silu, [pb[bs[half]]], [z])
                    tt("dve", oT[:, h, :], oT[:, h, :], z[:], ALU.mult, [oT, z], [oT])
                proj_chunk(pj, uTo, chunk0 + h, ev)
        wos = [sb("wos%d" % i, [8, 128], F32) for i in range(4)]
        wobf = [sb("wobf%d" % i, [8, 128], BF16) for i in range(4)]
        sg = [sb("sg%d" % i, [1024], F32) for i in range(2)]
        t12 = [sb("t12%d" % i, [1024], F32) for i in range(2)]
        for fc in range(16):
            wa_s, wb_s = wos[(fc % 2) * 2], wos[(fc % 2) * 2 + 1]
            wa, wb = wobf[(fc % 2) * 2], wobf[(fc % 2) * 2 + 1]
            dma(wa_s[:], woa[fc * 128:(fc + 1) * 128, :].rearrange("p (c f) -> p c f", c=8), [], [wa_s], wa_s.name)
            dma2(1, wb_s[:], wob[fc * 128:(fc + 1) * 128, :].rearrange("p (c f) -> p c f", c=8), [], [wb_s], wb_s.name)
            cp("act", wa[:], wa_s[:], [wa_s], [wa])
            cp("dve", wb[:], wb_s[:], [wb_s], [wb])
            for half in range(2):
                for c in range(8):
                    mm(pb[half][:, :], wa[:, c, :], oaT[:, c, half * 512:(half + 1) * 512], c == 0, c == 7, [wa, oaT], [pb[half]])
            for half in range(2):
                for c in range(8):
                    mm(pb[2 + half][:, :], wb[:, c, :], obT[:, c, half * 512:(half + 1) * 512], c == 0, c == 7, [wb, obT], [pb[2 + half]])

            def ev_ga(bs):
                act(sg[0][:], psv(4, 2), AF.Sigmoid, pbs(4, 2), [sg[0]])
                tt("dve", t12[0][:], sg[0][:], psv(0, 2), ALU.mult, [sg[0]] + pbs(0, 2), [t12[0]])

            def ev_gb(bs, fc=fc):
                act(sg[1][:], psv(6, 2), AF.Sigmoid, pbs(6, 2), [sg[1]])
                tt("dve", t12[1][:], sg[1][:], psv(2, 2), ALU.mult, [sg[1]] + pbs(2, 2), [t12[1]])
                tt("dve", mixT[:, fc, :], t12[0][:], t12[1][:], ALU.add, [t12[0], t12[1]], [mixT])

            if fc == 0:
                pre_ga = proj_prefetch(pj, 40)
                pre_gb = proj_prefetch(pj, 56)
            nxt = {}

            def ev_ga2(bs, fc=fc):
                nxt["ga"] = proj_prefetch(pj, 40 + fc + 1) if fc + 1 < 16 else None
                ev_ga(bs)

            def ev_gb2(bs, fc=fc):
                nxt["gb"] = proj_prefetch(pj, 56 + fc + 1) if fc + 1 < 16 else None
                ev_gb(bs)

            proj_chunk(pj, uTo, 40 + fc, ev_ga2, bank_sets=((4, 5),), pre=pre_ga)
            proj_chunk(pj, uTo, 56 + fc, ev_gb2, bank_sets=((6, 7),), pre=pre_gb)
            pre_ga, pre_gb = nxt["ga"], nxt["gb"]
        P.barrier()
        free_proj(pj)
        AR.free(uTo, *zs, *wos, *wobf, *sg, *t12, oaT, obT)
        woutb = sb("woutb", [16, 2048], BF16)
        wsts = [sb("wsts%d" % i, [2048], F32) for i in range(2)]
        postwt = sb("postwt", [2048], F32)
        dma(postwt[:], postw, [], [postwt], "postwt")
        for c in range(16):
            s = wsts[c % 2]
            dma2(c, s[:], wout[c * 128:(c + 1) * 128, :], [], [s], s.name)
            cp("act" if c % 2 == 0 else "dve", woutb[:, c, :], s[:], [s], [woutb])
        xrs = [sb("xr%d" % i, [2048], F32) for i in range(2)]
        yo = [sb("yo%d" % i, [2048], F32) for i in range(2)]
        sts = [sb("st%d" % i, [16], F32) for i in range(2)]
        for j in range(8):
            b0 = 4 * (j % 2)
            st, xr, yt = sts[j % 2], xrs[j % 2], yo[j % 2]
            dma(xr[:], xq[j * 128:(j + 1) * 128, :], [], [xr], xr.name)
            for c in range(16):
                for n in range(4):
                    mm(pb[b0 + n][:, :], mixT[:, c, j * 128:(j + 1) * 128], woutb[:, c, n * 512:(n + 1) * 512], c == 0, c == 15, [mixT, woutb], [pb[b0 + n]])
            P.add("act", lambda e, st=st, b0=b0: e.activation(out=junk[:], in_=psv(b0, 4), func=AF.Square, accum_out=st[:, 0:1]), reads=pbs(b0, 4), writes=[st])
            act(st[:, 1:2], st[:, 0:1], AF.Sqrt, [st, epsb], [st], scale=1.0 / 2048, bias=epsb[:])
            recip(st[:, 2:3], st[:, 1:2], [st], [st])
            stt(yt[:], psv(b0, 4), st[:, 2:3], postwt[:], ALU.mult, ALU.mult, pbs(b0, 4) + [st, postwt], [yt])
            tt("dve", yt[:], yt[:], xr[:], ALU.add, [yt, xr], [yt])
            dmas(y[j * 128:(j + 1) * 128, :], yt[:], [yt], [], yt.name + "_st")
        P.emit(es)
    return nc


def _t5_bucket(rel):
    nb, me = 16, 8
    ret = np.where(rel > 0, nb, 0)
    n = np.abs(rel)
    nf = np.maximum(n, 1).astype(np.float32)
    large = me + (np.log(nf / np.float32(me)) / np.float32(np.log(16.0)) * np.float32(nb - me)).astype(np.int32)
    large = np.minimum(large, nb - 1)
    return ret + np.where(n < me, n, large)


_NC_CACHE = {}


def _host_inputs(inputs):
    f = lambda a: np.ascontiguousarray(np.asarray(a, dtype=np.float32))
    x = f(inputs["x"])
    meta = f(inputs["meta_tokens"])
    rel_bias = f(inputs["rel_bias"])
    w_in = f(inputs["w_in"])[0]
    o = np.cumsum([0, 1024, 256, 1024, 1024, 64, 16, 1024, 1024, 1024, 1024, 2048, 2048])
    col = lambda i: w_in[:, o[i]:o[i + 1]]
    wk = np.ascontiguousarray(np.concatenate([col(1), col(4), col(7), col(8)], axis=1))
    wq = np.concatenate([col(3), col(6), col(0), col(2), col(9), col(10), col(11)], axis=1)
    wq = np.ascontiguousarray(wq.reshape(16, 128, 72, 128).transpose(2, 1, 0, 3)).reshape(72 * 128, 2048)

    def chunk_major8(w):
        return np.ascontiguousarray(w.reshape(8, 128, 16, 128).transpose(2, 1, 0, 3)).reshape(16 * 128, 1024)
    wiw = np.ascontiguousarray(col(5))
    w_uk = f(inputs["w_uk"])[0]
    w_uv = f(inputs["w_uv"])[0]
    shared = {
        "wk": wk, "wq": wq, "wiw": wiw,
        "wukT": np.ascontiguousarray(w_uk.transpose(2, 1, 0).reshape(128, 2048)),
        "wuv": np.ascontiguousarray(w_uv.reshape(256, 1024)),
        "woa": chunk_major8(f(inputs["w_o_a"])[0]), "wob": chunk_major8(f(inputs["w_o_b"])[0]), "wout": f(inputs["w_out"])[0],
        "prewT": np.ascontiguousarray(f(inputs["pre_norm_w"])[0].reshape(16, 128).T),
        "kvw": np.ascontiguousarray(np.broadcast_to(f(inputs["kv_norm_w"])[0][None], (128, 256))),
        "ikw": np.ascontiguousarray(np.broadcast_to(np.tile(f(inputs["idx_k_norm_w"])[0], 2)[None], (128, 128))),
        "ikb": np.ascontiguousarray(np.broadcast_to(np.tile(f(inputs["idx_k_norm_b"])[0], 2)[None], (128, 128))),
        "lamp": np.ascontiguousarray(np.broadcast_to(f(inputs["diff_lambda"])[0].reshape(1, 256), (128, 256))),
        "subw": np.ascontiguousarray(f(inputs["diff_subln_w"])[0].reshape(128, 1)),
        "postw": np.ascontiguousarray(np.broadcast_to(f(inputs["post_norm_w"])[0][None], (128, 2048))),
        "ident": np.eye(128, dtype=np.float32),
        "ca": np.ascontiguousarray(np.broadcast_to(rel_bias[15, 0:8][None], (128, 8))),
        "cb": np.ascontiguousarray(np.broadcast_to(rel_bias[15, 8:16][None], (128, 8))),
    }
    s = np.arange(128)[:, None]
    t = np.arange(128)[None, :]
    in_maps = []
    for core in range(8):
        b, qq = core // 4, core % 4
        blocks = [4 * j + qq for j in range(8)]
        xk = np.zeros((4224, 2048), np.float32)
        xk[:4096] = x[b]
        xk[4096:4112] = meta
        xq = np.ascontiguousarray(x[b].reshape(32, 128, 2048)[blocks].reshape(1024, 2048))
        bta = np.zeros((128, 5, 8, 128), np.float32)
        btb = np.zeros((128, 8, 5, 128), np.float32)
        madd = np.zeros((128, 5, 128), np.float32)
        for r in range(5):
            dblk = r - 1 - qq
            rel = 128 * dblk + s - t
            bk = _t5_bucket(rel)
            bta[:, r, :, :] = rel_bias[bk][:, :, 0:8].transpose(0, 2, 1)
            btb[:, :, r, :] = rel_bias[bk][:, :, 8:16].transpose(0, 2, 1)
            allowed = ((128 * dblk + s) // 64) <= (t // 64)
            madd[:, r, :] = np.where(allowed, 0.0, NEG)
        mk = np.zeros((128, 512), np.float32)
        for rr in range(4):
            dblk = rr - qq
            tq = np.arange(128)[:, None]
            sk = np.arange(128)[None, :]
            allowed = ((128 * dblk + sk) // 64) <= (tq // 64)
            mk[:, rr * 128:(rr + 1) * 128] = np.where(allowed, 0.0, NEG)
        relm = np.arange(16)[:, None] - 16 - (128 * qq + t)
        bkm = _t5_bucket(relm)
        btam = np.ascontiguousarray(rel_bias[bkm][:, :, 0:8].transpose(0, 2, 1)).reshape(16, 1024)
        btbm = np.ascontiguousarray(rel_bias[bkm][:, :, 8:16].transpose(0, 2, 1)).reshape(16, 1024)
        m = dict(shared)
        m.update({
            "xk": xk, "xq": xq,
            "bta": np.ascontiguousarray(bta.reshape(128, 5120)), "btb": np.ascontiguousarray(btb.reshape(128, 5120)),
            "btam": btam, "btbm": btbm,
            "madd": np.ascontiguousarray(madd.reshape(128, 640)), "mk": mk,
        })
        in_maps.append(m)
    return in_maps


def kernel(**inputs):
    in_maps = _host_inputs(inputs)
    if "nc" not in _NC_CACHE:
        _NC_CACHE["nc"] = build_program()
    nc = _NC_CACHE["nc"]
    res = run_bass_kernel_spmd(nc, in_maps, core_ids=list(range(8)))
    out = np.zeros((2, 4096, 2048), np.float32)
    o4 = out.reshape(2, 32, 128, 2048)
    for core in range(8):
        b, qq = core // 4, core % 4
        yy = np.asarray(res.results[core]["y"]).reshape(8, 128, 2048)
        for j in range(8):
            o4[b, 4 * j + qq] = yy[j]
    return out
```
